# Optimizing a Trainium2 kernel written in Bass

```python
import jax, jax.numpy as jnp
from jax import lax
import numpy as np

D_MODEL = 1024
BATCH = 8
SEQ = 4096
DEPTH = 2

PLE_DIM = 256
N_EVEN = (DEPTH + 1) // 2
N_ODD = DEPTH // 2
RG_WIDTH = 512
RG_BLOCKS = 8
RG_BLOCK = RG_WIDTH // RG_BLOCKS
RG_CONV = 4
RG_C = 8.0
ML_HEADS = 4
ML_HEAD_DIM = 128
ML_WIDTH = ML_HEADS * ML_HEAD_DIM
ML_CONV = 4
ML_CHUNK = 64
HY_IN = 2 * RG_WIDTH + 4 * ML_WIDTH + 2 * ML_HEADS
HY_MIX = RG_WIDTH + ML_WIDTH
AT_HEADS = 16
AT_KV_HEADS = 4
AT_HEAD_DIM = 64
WINDOW = 128
ROPE_THETA = 10000.0
AT_QKV = (AT_HEADS + 2 * AT_KV_HEADS) * AT_HEAD_DIM
FF_DIM = 3 * D_MODEL
FF_CONV = 3
EPS = 1e-6

kernel_name = 'hybrid_rglru_mlstm_swa_trunk'


def rms_norm(x, g):
    xf = x.astype(jnp.float32)
    y = xf * lax.rsqrt(jnp.mean(xf * xf, axis=-1, keepdims=True) + EPS)
    return (y * g.astype(jnp.float32)).astype(x.dtype)


def causal_dwconv(x, w, b):
    k_w = w.shape[0]
    s = x.shape[1]
    xp = jnp.pad(x, ((0, 0), (k_w - 1, 0), (0, 0)))
    y = xp[:, 0:s] * w[0]
    for j in range(1, k_w):
        y = y + xp[:, j:j + s] * w[j]
    return y + b


def _linear_recurrence_combine(left, right):
    a_l, b_l = left
    a_r, b_r = right
    return a_l * a_r, a_r * b_l + b_r


def rg_lru(x, w_a, b_a, w_x, b_x, lam):
    f32 = jnp.float32
    bsz, s, _ = x.shape
    xf = x.astype(f32)
    xb = xf.reshape(bsz, s, RG_BLOCKS, RG_BLOCK)
    r = jax.nn.sigmoid(jnp.einsum('bsni,nij->bsnj', xb, w_a.astype(f32)).reshape(bsz, s, RG_WIDTH) + b_a.astype(f32))
    i = jax.nn.sigmoid(jnp.einsum('bsni,nij->bsnj', xb, w_x.astype(f32)).reshape(bsz, s, RG_WIDTH) + b_x.astype(f32))
    log_a = -RG_C * r * jax.nn.softplus(-lam.astype(f32))
    a = jnp.exp(log_a)
    u = jnp.sqrt(-jnp.expm1(2.0 * log_a)) * (i * xf)
    _, h = lax.associative_scan(_linear_recurrence_combine, (a, u), axis=1)
    return h


def mlstm_chunkwise(q, k, v, i_pre, f_pre):
    f32 = jnp.float32
    bsz, s, nh, dh = q.shape
    nc = s // ML_CHUNK

    def to_chunks(t):
        return t.astype(f32).reshape(bsz, nc, ML_CHUNK, nh, dh).transpose(0, 3, 1, 2, 4)

    def gate_chunks(t):
        return t.astype(f32).reshape(bsz, nc, ML_CHUNK, nh).transpose(0, 3, 1, 2)

    qc = to_chunks(q)
    kc = to_chunks(k) * (dh ** -0.5)
    vc = to_chunks(v)
    log_i = gate_chunks(i_pre)
    log_f = jax.nn.log_sigmoid(gate_chunks(f_pre))
    b = jnp.cumsum(log_f, axis=-1)
    b_last = b[..., -1]

    g = b_last[..., None] - b + log_i
    m_loc = jnp.max(g, axis=-1)
    w_loc = jnp.exp(g - m_loc[..., None])
    c_loc = jnp.einsum('bhcl,bhcld,bhcle->bhcde', w_loc, kc, vc)
    n_loc = jnp.einsum('bhcl,bhcld->bhcd', w_loc, kc)

    def step(carry, inp):
        c_st, n_st, m_st = carry
        c_l, n_l, m_l, b_l = inp
        m_new = jnp.maximum(b_l + m_st, m_l)
        a_prev = jnp.exp(b_l + m_st - m_new)
        a_loc = jnp.exp(m_l - m_new)
        c_new = a_prev[..., None, None] * c_st + a_loc[..., None, None] * c_l
        n_new = a_prev[..., None] * n_st + a_loc[..., None] * n_l
        return (c_new, n_new, m_new), (c_st, n_st, m_st)

    init = (jnp.zeros((bsz, nh, dh, dh), f32), jnp.zeros((bsz, nh, dh), f32), jnp.full((bsz, nh), -jnp.inf, f32))
    xs = (jnp.moveaxis(c_loc, 2, 0), jnp.moveaxis(n_loc, 2, 0), jnp.moveaxis(m_loc, 2, 0), jnp.moveaxis(b_last, 2, 0))
    _, (c_prev, n_prev, m_prev) = lax.scan(step, init, xs)
    c_prev = jnp.moveaxis(c_prev, 0, 2)
    n_prev = jnp.moveaxis(n_prev, 0, 2)
    m_prev = jnp.moveaxis(m_prev, 0, 2)

    idx = jnp.arange(ML_CHUNK)
    causal = idx[:, None] >= idx[None, :]
    d_log = b[..., :, None] - b[..., None, :] + log_i[..., None, :]
    d_log = jnp.where(causal, d_log, -jnp.inf)
    inter_log = b + m_prev[..., None]
    m_t = jnp.maximum(inter_log, jnp.max(d_log, axis=-1))
    w_intra = jnp.exp(d_log - m_t[..., None])
    a_inter = jnp.exp(inter_log - m_t)
    s_qk = jnp.einsum('bhctd,bhcsd->bhcts', qc, kc) * w_intra
    num = jnp.einsum('bhcts,bhcse->bhcte', s_qk, vc) + a_inter[..., None] * jnp.einsum('bhctd,bhcde->bhcte', qc, c_prev)
    den = jnp.sum(s_qk, axis=-1) + a_inter * jnp.einsum('bhctd,bhcd->bhct', qc, n_prev)
    h = num / jnp.maximum(jnp.abs(den), jnp.exp(-m_t))[..., None]
    return h.transpose(0, 2, 3, 1, 4).reshape(bsz, s, nh, dh)


def rglru_mlstm_mixer(h, w_in, b_in, rg_conv_w, rg_conv_b, rg_w_a, rg_b_a, rg_w_x, rg_b_x, rg_lambda,
                      ml_conv_w, ml_conv_b, ml_norm, w_out):
    f32 = jnp.float32
    bsz, s, _ = h.shape
    z = h @ w_in + b_in
    cuts = [RG_WIDTH, 2 * RG_WIDTH, 2 * RG_WIDTH + ML_WIDTH, 2 * RG_WIDTH + 2 * ML_WIDTH,
            2 * RG_WIDTH + 3 * ML_WIDTH, 2 * RG_WIDTH + 4 * ML_WIDTH, 2 * RG_WIDTH + 4 * ML_WIDTH + ML_HEADS]
    x_rg, g_rg, q, k, v, o, i_pre, f_pre = jnp.split(z, cuts, axis=-1)
    xc = causal_dwconv(x_rg, rg_conv_w, rg_conv_b)
    y_rg = rg_lru(xc, rg_w_a, rg_b_a, rg_w_x, rg_b_x, rg_lambda) * jax.nn.gelu(g_rg.astype(f32), approximate=True)
    qk = jax.nn.silu(causal_dwconv(jnp.concatenate([q, k], axis=-1), ml_conv_w, ml_conv_b))
    q, k = jnp.split(qk, 2, axis=-1)
    hm = mlstm_chunkwise(q.reshape(bsz, s, ML_HEADS, ML_HEAD_DIM), k.reshape(bsz, s, ML_HEADS, ML_HEAD_DIM),
                         v.reshape(bsz, s, ML_HEADS, ML_HEAD_DIM), i_pre, f_pre)
    hm = rms_norm(hm, ml_norm.reshape(ML_HEADS, ML_HEAD_DIM))
    y_ml = jax.nn.sigmoid(o.astype(f32)) * hm.reshape(bsz, s, ML_WIDTH)
    y = jnp.concatenate([y_rg, y_ml], axis=-1).astype(h.dtype)
    return y @ w_out


def rope_tables(positions):
    half = AT_HEAD_DIM // 2
    inv_freq = ROPE_THETA ** (-jnp.arange(half, dtype=jnp.float32) * (2.0 / AT_HEAD_DIM))
    ang = positions.astype(jnp.float32)[..., None] * inv_freq
    return jnp.cos(ang)[:, :, None, :], jnp.sin(ang)[:, :, None, :]


def apply_rope(x, cos, sin):
    xf = x.astype(jnp.float32)
    x1, x2 = jnp.split(xf, 2, axis=-1)
    return jnp.concatenate([x1 * cos - x2 * sin, x2 * cos + x1 * sin], axis=-1)


def banded_sink_attention(q, k, v, sinks):
    f32 = jnp.float32
    bsz, s, _, dh = q.shape
    nb = s // WINDOW
    grp = AT_HEADS // AT_KV_HEADS
    qb = q.astype(f32).reshape(bsz, nb, WINDOW, AT_KV_HEADS, grp, dh)

    def key_window(t):
        tb = t.astype(f32).reshape(bsz, nb, WINDOW, AT_KV_HEADS, dh)
        prev = jnp.pad(tb, ((0, 0), (1, 0), (0, 0), (0, 0), (0, 0)))[:, :-1]
        return jnp.concatenate([prev, tb], axis=2)

    kw = key_window(k)
    vw = key_window(v)
    logits = jnp.einsum('bnqhgd,bnkhd->bnhgqk', qb, kw) * (dh ** -0.5)
    qi = jnp.arange(WINDOW)[:, None]
    kj = jnp.arange(2 * WINDOW)[None, :]
    band = (kj > qi) & (kj <= qi + WINDOW)
    blk = jnp.arange(nb)[:, None, None]
    valid = band[None] & ((blk > 0) | (kj >= WINDOW)[None])
    logits = jnp.where(valid[None, :, None, None], logits, -jnp.inf)
    sink = sinks.astype(f32).reshape(AT_KV_HEADS, grp)[:, :, None]
    m = jnp.maximum(jnp.max(logits, axis=-1), sink)
    probs = jnp.exp(logits - m[..., None])
    denom = jnp.sum(probs, axis=-1) + jnp.exp(sink - m)
    out = jnp.einsum('bnhgqk,bnkhd->bnhgqd', probs, vw) / denom[..., None]
    return out.transpose(0, 1, 4, 2, 3, 5).reshape(bsz, s, AT_HEADS * dh)


def swa_sink_mixer(h, positions, w_qkv, q_norm, k_norm, sinks, w_out):
    bsz, s, _ = h.shape
    qkv = h @ w_qkv
    q, k, v = jnp.split(qkv, [AT_HEADS * AT_HEAD_DIM, (AT_HEADS + AT_KV_HEADS) * AT_HEAD_DIM], axis=-1)
    q = q.reshape(bsz, s, AT_HEADS, AT_HEAD_DIM)
    k = k.reshape(bsz, s, AT_KV_HEADS, AT_HEAD_DIM)
    v = v.reshape(bsz, s, AT_KV_HEADS, AT_HEAD_DIM)
    cos, sin = rope_tables(positions)
    q = apply_rope(rms_norm(q, q_norm), cos, sin)
    k = apply_rope(rms_norm(k, k_norm), cos, sin)
    o = banded_sink_attention(q, k, v, sinks)
    return o.astype(h.dtype) @ w_out


def conv_ffn(h, w_up, conv_w, conv_b, w_down):
    g, u = jnp.split(h @ w_up, 2, axis=-1)
    g = causal_dwconv(g, conv_w, conv_b)
    return (jax.nn.gelu(g, approximate=True) * u) @ w_down


def _normal(key, shape, scale):
    return jax.random.normal(key, shape, jnp.float32) * scale


def setup_inputs(seed: int = 0) -> dict:
    key = jax.random.key(seed)
    ks = jax.random.split(key, 30)
    x = _normal(ks[0], (BATCH, SEQ, D_MODEL), 1.0)
    p = _normal(ks[1], (DEPTH, BATCH, SEQ, PLE_DIM), 1.0)
    offs = jax.random.randint(ks[2], (BATCH, 1), 0, 1024, dtype=jnp.int32)
    positions = offs + jnp.arange(SEQ, dtype=jnp.int32)[None, :]
    norm_mix = 1.0 + _normal(ks[3], (DEPTH, D_MODEL), 0.05)
    norm_ffn = 1.0 + _normal(ks[4], (DEPTH, D_MODEL), 0.05)
    norm_ple = 1.0 + _normal(ks[5], (DEPTH, D_MODEL), 0.05)
    hy_w_in = _normal(ks[6], (N_EVEN, D_MODEL, HY_IN), D_MODEL ** -0.5)
    hy_b_in = _normal(ks[7], (N_EVEN, HY_IN), 0.02)
    hy_b_in = hy_b_in.at[:, HY_IN - ML_HEADS:].add(jnp.linspace(3.0, 6.0, ML_HEADS))
    rg_conv_w = _normal(ks[8], (N_EVEN, RG_CONV, RG_WIDTH), RG_CONV ** -0.5)
    rg_conv_b = _normal(ks[9], (N_EVEN, RG_WIDTH), 0.02)
    rg_w_a = _normal(ks[10], (N_EVEN, RG_BLOCKS, RG_BLOCK, RG_BLOCK), RG_BLOCK ** -0.5)
    rg_b_a = _normal(ks[11], (N_EVEN, RG_WIDTH), 0.02)
    rg_w_x = _normal(ks[12], (N_EVEN, RG_BLOCKS, RG_BLOCK, RG_BLOCK), RG_BLOCK ** -0.5)
    rg_b_x = _normal(ks[13], (N_EVEN, RG_WIDTH), 0.02)
    a_c = jax.random.uniform(ks[14], (N_EVEN, RG_WIDTH), jnp.float32, 0.9, 0.999)
    a_base = a_c ** (1.0 / RG_C)
    rg_lambda = jnp.log(a_base) - jnp.log1p(-a_base)
    ml_conv_w = _normal(ks[15], (N_EVEN, ML_CONV, 2 * ML_WIDTH), ML_CONV ** -0.5)
    ml_conv_b = _normal(ks[16], (N_EVEN, 2 * ML_WIDTH), 0.02)
    ml_norm = 1.0 + _normal(ks[17], (N_EVEN, ML_WIDTH), 0.05)
    hy_w_out = _normal(ks[18], (N_EVEN, HY_MIX, D_MODEL), HY_MIX ** -0.5)
    at_w_qkv = _normal(ks[19], (N_ODD, D_MODEL, AT_QKV), D_MODEL ** -0.5)
    at_q_norm = 1.0 + _normal(ks[20], (N_ODD, AT_HEAD_DIM), 0.05)
    at_k_norm = 1.0 + _normal(ks[21], (N_ODD, AT_HEAD_DIM), 0.05)
    at_sinks = _normal(ks[22], (N_ODD, AT_HEADS), 0.5)
    at_w_out = _normal(ks[23], (N_ODD, AT_HEADS * AT_HEAD_DIM, D_MODEL), (AT_HEADS * AT_HEAD_DIM) ** -0.5)
    ff_w_up = _normal(ks[24], (DEPTH, D_MODEL, 2 * FF_DIM), D_MODEL ** -0.5)
    ff_conv_w = _normal(ks[25], (DEPTH, FF_CONV, FF_DIM), FF_CONV ** -0.5)
    ff_conv_b = _normal(ks[26], (DEPTH, FF_DIM), 0.02)
    ff_w_down = _normal(ks[27], (DEPTH, FF_DIM, D_MODEL), FF_DIM ** -0.5)
    ple_w_gate = _normal(ks[28], (DEPTH, D_MODEL, D_MODEL), D_MODEL ** -0.5)
    ple_w_proj = _normal(ks[29], (DEPTH, PLE_DIM, D_MODEL), PLE_DIM ** -0.5)
    return {'x': x, 'p': p, 'positions': positions,
            'norm_mix': norm_mix, 'norm_ffn': norm_ffn, 'norm_ple': norm_ple,
            'hy_w_in': hy_w_in, 'hy_b_in': hy_b_in,
            'rg_conv_w': rg_conv_w, 'rg_conv_b': rg_conv_b, 'rg_w_a': rg_w_a, 'rg_b_a': rg_b_a,
            'rg_w_x': rg_w_x, 'rg_b_x': rg_b_x, 'rg_lambda': rg_lambda,
            'ml_conv_w': ml_conv_w, 'ml_conv_b': ml_conv_b, 'ml_norm': ml_norm, 'hy_w_out': hy_w_out,
            'at_w_qkv': at_w_qkv, 'at_q_norm': at_q_norm, 'at_k_norm': at_k_norm, 'at_sinks': at_sinks,
            'at_w_out': at_w_out,
            'ff_w_up': ff_w_up, 'ff_conv_w': ff_conv_w, 'ff_conv_b': ff_conv_b, 'ff_w_down': ff_w_down,
            'ple_w_gate': ple_w_gate, 'ple_w_proj': ple_w_proj}


def reference(x, p, positions, norm_mix, norm_ffn, norm_ple, hy_w_in, hy_b_in,
              rg_conv_w, rg_conv_b, rg_w_a, rg_b_a, rg_w_x, rg_b_x, rg_lambda,
              ml_conv_w, ml_conv_b, ml_norm, hy_w_out,
              at_w_qkv, at_q_norm, at_k_norm, at_sinks, at_w_out,
              ff_w_up, ff_conv_w, ff_conv_b, ff_w_down, ple_w_gate, ple_w_proj):
    for layer in range(DEPTH):
        h = rms_norm(x, norm_mix[layer])
        if layer % 2 == 0:
            e = layer // 2
            mix = rglru_mlstm_mixer(h, hy_w_in[e], hy_b_in[e], rg_conv_w[e], rg_conv_b[e], rg_w_a[e], rg_b_a[e],
                                    rg_w_x[e], rg_b_x[e], rg_lambda[e], ml_conv_w[e], ml_conv_b[e], ml_norm[e],
                                    hy_w_out[e])
        else:
            o = layer // 2
            mix = swa_sink_mixer(h, positions, at_w_qkv[o], at_q_norm[o], at_k_norm[o], at_sinks[o], at_w_out[o])
        x = x + mix.astype(x.dtype)
        x = x + conv_ffn(rms_norm(x, norm_ffn[layer]), ff_w_up[layer], ff_conv_w[layer], ff_conv_b[layer],
                         ff_w_down[layer]).astype(x.dtype)
        gate = jax.nn.sigmoid(rms_norm(x, norm_ple[layer]) @ ple_w_gate[layer])
        x = x + (gate * (p[layer] @ ple_w_proj[layer])).astype(x.dtype)
    return x
```

```python
import numpy as np
import concourse.bass as bass
import concourse.mybir as mybir
from concourse.bass_utils import run_bass_kernel_spmd

F32 = mybir.dt.float32
BF16 = mybir.dt.bfloat16
I32 = mybir.dt.int32
AF = mybir.ActivationFunctionType
ALU = mybir.AluOpType

S = 4096
D = 1024
TB = 512
NT = 4
EPS = 1e-6
NSLOT = 4
SAME_ENG_SYNC = True
ENGS = ("pe", "act", "dve", "pool", "sp")
ACT_SET = {AF.Exp: "le", AF.Ln: "le", AF.Sigmoid: "sg", AF.Gelu_apprx_tanh: "ge", AF.Silu: "si", AF.Sqrt: "sq", AF.Sin: "sn"}


class Sched:
    GROUP_SEMS = ("setup", "setup2")
    LAT = 0.2
    PRIO = False
    SAME_LAT = 0.1
    ACT_BIAS = 60

    def __init__(self, nc):
        self.nc = nc
        self.ops = []
        self.sem = {}
        for e in ENGS:
            self._sem(e)

    def _sem(self, name):
        if name not in self.sem:
            self.sem[name] = self.nc.alloc_semaphore("s_" + name)
        return self.sem[name]

    def op(self, e, fn, r=(), w=(), dur=0.5):
        self.ops.append(dict(eng=e, fn=fn, r=list(r), w=list(w), kind="op", dur=dur))

    def dma(self, q, fn, sem, r=(), w=(), nbytes=0):
        self._sem(sem)
        self.ops.append(dict(eng=q, fn=fn, r=list(r), w=list(w), kind="dma", sem=sem, nbytes=nbytes, dur=0.0))

    def final_wait(self, e, keys):
        self.ops.append(dict(eng=e, fn=None, r=list(keys), w=[], kind="wait", dur=0.0))

    def finalize(self, reorder=True, keep=()):
        import heapq
        ops = self.ops
        n = len(ops)
        lastw, readers = {}, {}
        preds = [None] * n
        for i, o in enumerate(ops):
            ps = set()
            for k in o["r"]:
                if k in lastw:
                    ps.add(lastw[k])
            for k in o["w"]:
                if k in lastw:
                    ps.add(lastw[k])
                ps |= readers.get(k, set())
            ps.discard(i)
            preds[i] = ps
            for k in o["w"]:
                lastw[k] = i
                readers[k] = set()
            for k in o["r"]:
                readers.setdefault(k, set()).add(i)
        openg = {}
        for i, o in enumerate(ops):
            g = o.get("grp")
            if g is None:
                continue
            key = tuple(o["w"])
            if g[0] and not g[1]:
                openg[key] = [i]
            elif key in openg:
                openg[key].append(i)
                if g[1]:
                    mem = openg.pop(key)
                    ms = set(mem)
                    for m_ in mem[1:]:
                        preds[mem[0]] |= (preds[m_] - ms)
        succs = [[] for _ in range(n)]
        npend = [0] * n
        for i in range(n):
            npend[i] = len(preds[i])
            for p in preds[i]:
                succs[p].append(i)
        lp = [0.0] * n
        for i in range(n - 1, -1, -1):
            m_ = 0.0
            for sidx in succs[i]:
                if lp[sidx] > m_:
                    m_ = lp[sidx]
            o = ops[i]
            d_ = o["dur"] if o["kind"] == "op" else (2.0 + o.get("nbytes", 0) / 330e3 if o["kind"] == "dma" else 0.0)
            lp[i] = m_ + d_ + 0.2
        PRIO = self.PRIO
        order = {e: [] for e in ENGS}
        if not reorder:
            for i, o in enumerate(ops):
                order[o["eng"]].append(i)
        else:
            finish = [0.0] * n
            rt = [0.0] * n
            self.start = [0.0] * n
            self.why = [None] * n
            self.rtby = [None] * n
            lastop = {e: None for e in ENGS}
            tfree = {e: 0.0 for e in ENGS}
            ready = {e: [] for e in ENGS}
            dma_free = [0.0]
            for i in range(n):
                if npend[i] == 0:
                    heapq.heappush(ready[ops[i]["eng"]], (0.0, i))
            act_set = [None]
            ACT_BIAS = self.ACT_BIAS
            self.n_act_switch = 0
            pe_lock = [None]
            nxt = {e: 0 for e in ENGS}
            plist = {e: [i for i in range(n) if ops[i]["eng"] == e] for e in ENGS}
            done = 0
            while done < n:
                best = None
                for e in ENGS:
                    h = ready[e]
                    if not h:
                        continue
                    te = tfree[e]
                    cand = None
                    if e == "pe" and pe_lock[0] is not None:
                        cand = None
                        for x in h:
                            if x[1] == pe_lock[0]:
                                cand = x
                                break
                        if cand is None:
                            continue
                        st = max(te, cand[0])
                    elif e in keep:
                        want = plist[e][nxt[e]]
                        cand = None
                        for x in h:
                            if x[1] == want:
                                cand = x
                                break
                        if cand is None:
                            continue
                        st = max(te, cand[0])
                    elif h[0][0] <= te:
                        tmp = []
                        while h and h[0][0] <= te:
                            tmp.append(heapq.heappop(h))
                        if e == "act":
                            def _k(x):
                                a_ = ops[x[1]].get("aset")
                                sw = 1 if (a_ is not None and a_ != act_set[0]) else 0
                                return (x[1] + (ACT_BIAS if sw else 0), x[1])
                            tmp.sort(key=_k)
                        elif PRIO:
                            tmp.sort(key=lambda x: (-lp[x[1]], x[1]))
                        else:
                            tmp.sort(key=lambda x: x[1])
                        cand = tmp[0]
                        for x in tmp[1:]:
                            heapq.heappush(h, (x[0], x[1]))
                        heapq.heappush(h, cand)
                        st = te
                    else:
                        cand = h[0]
                        st = cand[0]
                    if best is None or st < best[0] or (st == best[0] and cand[1] < best[2][1]):
                        best = (st, e, cand)
                st, e, cand = best
                h = ready[e]
                h.remove(cand)
                heapq.heapify(h)
                i = cand[1]
                o = ops[i]
                self.start[i] = st
                self.why[i] = ("eng", lastop[e]) if (st > rt[i] + 1e-9 and lastop[e] is not None) else ("dep", self.rtby[i])
                lastop[e] = i
                if e == "act" and o.get("aset") is not None and o["aset"] != act_set[0]:
                    act_set[0] = o["aset"]
                    st += 1.3
                    self.n_act_switch += 1
                if o["kind"] == "dma":
                    issue = 1.2 if e == "pool" else 0.15
                    tfree[e] = st + issue
                    s0 = max(st + issue, dma_free[0])
                    dma_free[0] = s0 + o["nbytes"] / 330e3
                    finish[i] = dma_free[0] + 2.0
                else:
                    finish[i] = st + o["dur"]
                    tfree[e] = finish[i]
                if e == "pe":
                    g = o.get("grp")
                    if g is not None and not g[1]:
                        j = i + 1
                        while not (ops[j]["eng"] == "pe" and ops[j]["w"] == o["w"]):
                            j += 1
                        pe_lock[0] = j
                    else:
                        pe_lock[0] = None
                order[e].append(i)
                nxt[e] += 1
                done += 1
                for sidx in succs[i]:
                    if ops[sidx]["eng"] == e and o["kind"] != "dma":
                        lat = 0.0 if e == "pe" else self.SAME_LAT
                    else:
                        lat = self.LAT
                    v = finish[i] + lat
                    if v > rt[sidx]:
                        rt[sidx] = v
                        self.rtby[sidx] = i
                    npend[sidx] -= 1
                    if npend[sidx] == 0:
                        heapq.heappush(ready[ops[sidx]["eng"]], (rt[sidx], sidx))
            self.est_makespan = max(finish) if n else 0.0
            self.finish = finish
        tok = [None] * n
        cnt = {k: 0 for k in self.sem}
        for e in ENGS:
            for i in order[e]:
                o = ops[i]
                if o["kind"] == "op":
                    cnt[e] += 1
                    tok[i] = (e, cnt[e])
                elif o["kind"] == "dma":
                    cnt[o["sem"]] += 16
                    tok[i] = (o["sem"], cnt[o["sem"]])
        for i, o in enumerate(ops):
            if o["kind"] == "dma" and o["sem"] in self.GROUP_SEMS:
                tok[i] = (o["sem"], cnt[o["sem"]])
        self.streams = {e: [] for e in ENGS}
        for e in ENGS:
            seen = {}
            for i in order[e]:
                o = ops[i]
                need = {}
                for p in preds[i]:
                    t = tok[p]
                    if t is None:
                        continue
                    if need.get(t[0], 0) < t[1]:
                        need[t[0]] = t[1]
                waits = []
                for sname, v in need.items():
                    if sname == e and (e == "pe" or not SAME_ENG_SYNC):
                        continue
                    if seen.get(sname, 0) >= v:
                        continue
                    seen[sname] = v
                    waits.append((sname, v))
                inc = None
                if o["kind"] == "op":
                    inc = (e, 1)
                elif o["kind"] == "dma":
                    inc = (o["sem"], 16)
                self.streams[e].append((waits, o["fn"], inc))

    def replay(self, e, eng):
        for waits, fn, inc in self.streams[e]:
            for s, v in waits:
                eng.wait_ge(self.sem[s], v)
            if fn is None:
                continue
            ins = fn(eng)
            ins.then_inc(self.sem[inc[0]], inc[1])


def _nfree(ap):
    n = 1
    for d in ap.shape[1:]:
        n *= int(d)
    return n


def _fm(v):
    v = np.asarray(v, np.float32)
    c = v.shape[0] // 128
    return np.ascontiguousarray(v.reshape(c, 128).T)


def _taps(w):
    w = np.asarray(w, np.float32)
    k, c = w.shape
    return np.ascontiguousarray(w.T.reshape(c // 128, 128, k).transpose(1, 0, 2).reshape(128, -1))


COLS = {}
_off = 0
for _n, _w in [("nmix0", 8), ("nffn0", 8), ("nple0", 8), ("nmix1", 8), ("nffn1", 8), ("nple1", 8),
               ("bin", 16), ("rgcw", 16), ("rgcb", 4), ("rgba", 4), ("rgbx", 4), ("rglam", 4),
               ("mlcw", 32), ("mlcb", 8), ("ffcw0", 72), ("ffcw1", 72), ("ffcb0", 24), ("ffcb1", 24),
               ("qn", 1), ("kn", 1), ("invf", 1)]:
    COLS[_n] = (_off, _w)
    _off += _w
NCOLS = _off
ROWS = {"bvo": (0, 1024), "bif": (1024, 8), "mln": (1032, 512), "snk": (1544, 16)}
NROWS = 1560
CONS = {"ident": 0, "ones": 128, "tri": 256, "mprev": 384, "rt": 512, "hones": 640}
NCONS = 768


def host_tables(inp):
    cols = np.zeros((128, NCOLS), np.float32)

    def put(name, arr):
        o, w = COLS[name]
        cols[:, o:o + w] = np.asarray(arr, np.float32).reshape(128, w)

    for l in range(2):
        put("nmix%d" % l, _fm(inp["norm_mix"][l]))
        put("nffn%d" % l, _fm(inp["norm_ffn"][l]))
        put("nple%d" % l, _fm(inp["norm_ple"][l]))
        put("ffcw%d" % l, _taps(inp["ff_conv_w"][l]))
        put("ffcb%d" % l, _fm(inp["ff_conv_b"][l]))
    b_in = np.asarray(inp["hy_b_in"][0], np.float32)
    put("bin", _fm(b_in[0:2048]))
    put("rgcw", _taps(inp["rg_conv_w"][0]))
    put("rgcb", _fm(inp["rg_conv_b"][0]))
    put("rgba", _fm(inp["rg_b_a"][0]))
    put("rgbx", _fm(inp["rg_b_x"][0]))
    put("rglam", _fm(inp["rg_lambda"][0]))
    put("mlcw", _taps(inp["ml_conv_w"][0]))
    put("mlcb", _fm(inp["ml_conv_b"][0]))
    put("qn", np.tile(np.asarray(inp["at_q_norm"][0], np.float32), 2))
    put("kn", np.tile(np.asarray(inp["at_k_norm"][0], np.float32), 2))
    invf = (np.float32(10000.0) ** (-np.arange(32, dtype=np.float32) * np.float32(2.0 / 64))).astype(np.float32)
    put("invf", np.tile(invf, 4))

    rows = np.zeros((128, NROWS), np.float32)
    rows[:, 0:1024] = b_in[2048:3072][None, :]
    rows[:, 1024:1032] = b_in[3072:3080][None, :]
    rows[:, 1032:1544] = np.asarray(inp["ml_norm"][0], np.float32)[None, :]
    rows[:, 1544:1560] = np.asarray(inp["at_sinks"][0], np.float32)[None, :]

    cons = np.zeros((128, NCONS), np.float32)
    i = np.arange(128)
    cons[:, 0:128] = np.eye(128, dtype=np.float32)
    cons[:, 128:256] = 1.0
    cons[:, 256:384] = (i[:, None] <= i[None, :]).astype(np.float32)
    cons[:, 384:512] = (i[:, None] > i[None, :]).astype(np.float32)
    rt = np.zeros((128, 128), np.float32)
    for blk in range(2):
        for j in range(32):
            rt[blk * 64 + j + 32, blk * 64 + j] = -1.0
            rt[blk * 64 + j, blk * 64 + j + 32] = 1.0
    cons[:, 512:640] = rt
    cons[:, 640:768] = (i[:, None] // 64 == i[None, :] // 64).astype(np.float32)

    def bdiag(w):
        w = np.asarray(w, np.float32)
        out = np.zeros((128, 4, 128), np.float32)
        for c in range(4):
            for j in range(2):
                out[j * 64:(j + 1) * 64, c, j * 64:(j + 1) * 64] = w[2 * c + j]
        return out

    wqkv = np.asarray(inp["at_w_qkv"][0], np.float32)
    wk = wqkv[:, 1024:1280]
    wkd = np.concatenate([np.concatenate([wk[:, g * 64:(g + 1) * 64]] * 2, axis=1) for g in range(4)], axis=1)
    shared = {
        "cols": cols, "rows": rows, "cons": cons,
        "bda": bdiag(inp["rg_w_a"][0]), "bdx": bdiag(inp["rg_w_x"][0]),
        "wif": np.ascontiguousarray(np.asarray(inp["hy_w_in"][0], np.float32)[:, 3072:3080]),
        "w_in": np.ascontiguousarray(np.asarray(inp["hy_w_in"][0], np.float32)),
        "hy_w_out": np.ascontiguousarray(np.asarray(inp["hy_w_out"][0], np.float32)),
        "wq": np.ascontiguousarray(wqkv[:, 0:1024]),
        "wkd": np.ascontiguousarray(wkd),
        "wv": np.ascontiguousarray(wqkv[:, 1280:1536]),
        "at_w_out": np.ascontiguousarray(np.asarray(inp["at_w_out"][0], np.float32)),
        "ff_w_up": np.ascontiguousarray(np.asarray(inp["ff_w_up"], np.float32)),
        "ff_w_down": np.ascontiguousarray(np.asarray(inp["ff_w_down"], np.float32)),
        "ple_w_gate": np.ascontiguousarray(np.asarray(inp["ple_w_gate"], np.float32)),
        "ple_w_proj": np.ascontiguousarray(np.asarray(inp["ple_w_proj"], np.float32)),
    }
    return shared


class Prog:
    def __init__(self, nblk=8, nstage=6, skip=()):
        self.skip = set(skip)
        self.debug = False
        self.reorder = True
        self.keep = ()
        self.dbgkeys = []
        self.nblk = nblk
        self.nstage = nstage
        nc = self.nc = bass.Bass("TRN2", target_bir_lowering=False)
        self.s = Sched(nc)
        dt = nc.dram_tensor
        self.d = {}
        for name, shape, ty in [
            ("xT", [D, S], F32), ("pT", [2, 256, S], F32), ("pos", [1, S], I32),
            ("cols", [128, NCOLS], F32), ("rows", [128, NROWS], F32), ("cons", [128, NCONS], F32),
            ("bda", [128, 4, 128], F32), ("bdx", [128, 4, 128], F32), ("wif", [D, 8], F32),
            ("w_in", [D, 3080], F32), ("hy_w_out", [D, D], F32),
            ("wq", [D, 1024], F32), ("wkd", [D, 512], F32), ("wv", [D, 256], F32), ("at_w_out", [D, D], F32),
            ("ff_w_up", [2, D, 6144], F32), ("ff_w_down", [2, 3072, D], F32),
            ("ple_w_gate", [2, D, D], F32), ("ple_w_proj", [2, 256, D], F32),
        ]:
            self.d[name] = dt(name, shape, ty, kind="ExternalInput").ap()
        self.d["outT"] = dt("outT", [D, S], F32, kind="ExternalOutput").ap()
        self._n = 0
        self.rot = {}
        self.bankc = 0
        self.alloc()
        self.loads = []
        self.nload = 0
        self.nissued = 0

    def sb(self, shape, ty=F32, name=None):
        self._n += 1
        return self.nc.alloc_sbuf_tensor("%s_%d" % (name or "t", self._n), list(shape), ty).ap()

    def alloc(self):
        sb = self.sb
        self.ps = [self.nc.alloc_psum_tensor("ps%d" % i, [128, 512], F32).ap() for i in range(8)]
        self.cols = sb([128, NCOLS], F32, "cols")
        self.rows = sb([128, NROWS], F32, "rows")
        self.cons = sb([128, NCONS], F32, "cons")
        self.consb = sb([128, NCONS], BF16, "consb")
        self.bda = sb([128, 4, 128], BF16, "bda")
        self.bdx = sb([128, 4, 128], BF16, "bdx")
        self.wif = sb([128, 8, 8], BF16, "wif")
        self.misc = sb([128, 16], F32, "misc")
        self.cb2 = sb([128, 12], F32, "cb2")
        self.cA = sb([128, 8], F32, "cA")
        self.esink = sb([128, 16], F32, "esink")
        self.mask4 = sb([128, 512], BF16, "mask4")
        self.x = [sb([128, 8, TB], F32, "x%d" % i) for i in range(2)]
        self.h = sb([128, 8, TB], BF16, "h")
        self.rstd = sb([128, TB], F32, "rstd")
        self.slots = [sb([128, 4096], BF16, "ws%d" % i) for i in range(NSLOT)]
        self.yfm = sb([128, 8, TB], BF16, "yfm")
        self.tf = [sb([128, 520], F32, "tf%d" % i) for i in range(8)]
        self.cvin = [sb([128, 520], F32, "cvin%d" % i) for i in range(2)]
        self.cvacc = [sb([128, 520], F32, "cvacc%d" % i) for i in range(2)]
        self.tb = [sb([128, 520], BF16, "tb%d" % i) for i in range(4)]
        self.rg_halo = sb([128, 4, 3], F32, "rghalo")
        self.rg_h = sb([128, 4], F32, "rgh")
        self.ml_halo = sb([128, 8, 3], F32, "mlhalo")
        self.qk8 = sb([128, 8, TB], BF16, "qk8")
        self.qfm = self.qk8[:, 0:4, :]
        self.kfm = self.qk8[:, 4:8, :]
        self.qrope = self.qk8
        self.vaug = sb([128, NT, 4, 129], BF16, "vaug")
        self.og = sb([128, NT, 512], BF16, "og")
        self.ifs = sb([128, NT, 8], F32, "ifs")
        self.Cf = sb([128, 4, 129], F32, "Cf")
        self.Cb = sb([128, 4, 129], BF16, "Cb")
        self.sm = [sb([128, 64], F32, "sm%d" % i) for i in range(4)]
        self.Eb = [sb([128, 4, 128], F32, "Eb%d" % i) for i in range(2)]
        self.rhsall = [sb([128, 4, 128], F32, "rhsall%d" % i) for i in range(1)]
        self.hm = [sb([128, 4, 128], F32, "hm%d" % i) for i in range(2)]
        self.ytok = [sb([128, 1024], BF16, "ytok%d" % i) for i in range(2)]
        self.ff_halo = [sb([128, 24, 2], F32, "ffhalo%d" % l) for l in range(2)]
        self.hid = sb([128, 24, TB], BF16, "hid")
        self.xsq = self.hid[:, 0:8, :]
        self.pblk = [sb([128, 2, TB], BF16, "pblk%d" % i) for i in range(2)]
        self.posi = sb([128, TB], I32, "posi")
        self.cosb = sb([128, TB], F32, "cos")
        self.sinb = sb([128, TB], F32, "sin")
        self.kbuf = sb([128, 8, 128 + TB], BF16, "kbuf")
        self.vbuf = sb([128, NT + 1, 4, 65], BF16, "vbuf")

    def rotbuf(self, name, lst):
        i = self.rot.get(name, 0)
        self.rot[name] = i + 1
        j = i % len(lst)
        return lst[j], (name, j)

    def bank(self):
        i = self.bankc % 8
        self.bankc += 1
        return self.ps[i], ("ps", i)

    def col(self, name, j=0):
        o, w = COLS[name]
        return self.cols[:, o + j:o + j + 1]

    def con(self, name, bf=True):
        o = CONS[name]
        return (self.consb if bf else self.cons)[:, o:o + 128]

    def mm(self, out, lhsT, rhs, start, stop, r, w):
        n = _nfree(rhs)
        dur = max(0.06, n / 1940.0)
        if rhs.dtype == F32:
            dur *= 4
        self.s.op("pe", lambda e: e.matmul(out, lhsT=lhsT, rhs=rhs, start=start, stop=stop), r, w, dur=dur)
        self.s.ops[-1]["grp"] = (bool(start), bool(stop))

    def tr(self, out, in_, ident, r, w):
        self.s.op("pe", lambda e: e.transpose(out, in_, ident), r, w, dur=0.064 + 128 / 1940.0)

    def act(self, out, in_, func, r, w, bias=None, scale=None, accum=None):
        kw = {}
        if bias is not None:
            kw["bias"] = bias
        if scale is not None:
            kw["scale"] = scale
        if accum is not None:
            kw["accum_out"] = accum
        self.s.op("act", lambda e: e.activation(out=out, in_=in_, func=func, **kw), r, w, dur=0.22 + _nfree(out) * 0.0006)
        self.s.ops[-1]["aset"] = ACT_SET.get(func)

    def tt(self, out, a, b, op, r, w, eng="dve"):
        dur = (0.15 + _nfree(out) * 0.0022) if eng == "pool" else (0.1 + _nfree(out) * 0.00105)
        self.s.op(eng, lambda e: e.tensor_tensor(out=out, in0=a, in1=b, op=op), r, w, dur=dur)

    def ts(self, out, a, s1, s2, op0, op1, r, w, eng="dve"):
        if op1 is None:
            self.s.op(eng, lambda e: e.tensor_scalar(out=out, in0=a, scalar1=s1, scalar2=None, op0=op0), r, w,
                      dur=0.1 + _nfree(out) * 0.00105)
        else:
            self.s.op(eng, lambda e: e.tensor_scalar(out=out, in0=a, scalar1=s1, scalar2=s2, op0=op0, op1=op1), r, w,
                      dur=0.1 + _nfree(out) * 0.00105)

    def stt(self, out, a, sc, b, op0, op1, r, w):
        self.s.op("dve", lambda e: e.scalar_tensor_tensor(out=out, in0=a, scalar=sc, in1=b, op0=op0, op1=op1), r, w,
                  dur=0.1 + _nfree(out) * 0.00105)

    def cp(self, out, in_, r, w, eng="dve"):
        if eng == "act":
            self.s.op("act", lambda e: e.activation(out=out, in_=in_, func=AF.Identity), r, w, dur=0.22 + _nfree(out) * 0.0006)
        else:
            self.s.op(eng, lambda e: e.tensor_copy(out=out, in_=in_), r, w, dur=0.1 + _nfree(out) * 0.00105)

    def rcp(self, out, in_, r, w):
        self.s.op("dve", lambda e: e.reciprocal(out=out, in_=in_), r, w, dur=0.1 + _nfree(out) * 0.0024)

    def mset(self, out, val, w, eng="dve"):
        self.s.op(eng, lambda e: e.memset(out, val), (), w, dur=0.1 + _nfree(out) * 0.0005)

    def dma(self, q, out, in_, sem, r, w):
        nb = _nfree(out) * int(out.shape[0]) * (4 if in_.dtype == F32 else 2)
        self.s.dma(q, lambda e: e.dma_start(out=out, in_=in_), sem, r, w, nbytes=nb)

    def dump(self, name, ap, rkeys):
        if not self.debug:
            return
        t = self.nc.dram_tensor("dbg_" + name, list(ap.shape), ap.dtype, kind="ExternalOutput").ap()
        self.dma("sp", t, ap, "dbgsem_" + name, rkeys, [("dbg", name)])
        self.dbgkeys.append(("dbg", name))

    def plan_loads(self):
        L = []
        d = self.d

        def kview(ap2d, kc):
            return ap2d.rearrange("(k p) n -> p k n", p=128)

        for b in range(self.nblk):
            ns = self.nstage
            if ns >= 1 and "mix0" not in self.skip:
                for g in (2, 3, 4, 5, 0, 1):
                    L.append(("w_in%d" % g, kview(d["w_in"], 8)[:, :, g * 512:(g + 1) * 512], 8, 512))
                for g in range(2):
                    L.append(("hyout%d" % g, kview(d["hy_w_out"], 8)[:, :, g * 512:(g + 1) * 512], 8, 512))
            for l in range(2):
                if l == 1 and ns >= 4 and "mix1" not in self.skip:
                    for g in range(2):
                        L.append(("wq%d" % g, kview(d["wq"], 8)[:, :, g * 512:(g + 1) * 512], 8, 512))
                    L.append(("wkd", kview(d["wkd"], 8), 8, 512))
                    L.append(("wv", kview(d["wv"], 8), 8, 256))
                    for g in range(2):
                        L.append(("atout%d" % g, kview(d["at_w_out"], 8)[:, :, g * 512:(g + 1) * 512], 8, 512))
                if ns >= 2 + 3 * l and ("ffn%d" % l) not in self.skip:
                    up = kview(d["ff_w_up"][l], 8)
                    for g in range(6):
                        L.append(("upg%d_%d" % (l, g), up[:, :, g * 512:(g + 1) * 512], 8, 512))
                        L.append(("upu%d_%d" % (l, g), up[:, :, 3072 + g * 512:3072 + (g + 1) * 512], 8, 512))
                    dn = kview(d["ff_w_down"][l], 24)
                    for m in range(8):
                        L.append(("dn%d_%d" % (l, m), dn[:, :, m * 128:(m + 1) * 128], 24, 128))
                if ns >= 3 + 3 * l and ("ple%d" % l) not in self.skip:
                    pg = kview(d["ple_w_gate"][l], 8)
                    pp = kview(d["ple_w_proj"][l], 2)
                    for g in range(2):
                        L.append(("pp%d_%d" % (l, g), pp[:, :, g * 512:(g + 1) * 512], 2, 512))
                        L.append(("pg%d_%d" % (l, g), pg[:, :, g * 512:(g + 1) * 512], 8, 512))
        self.loads = L

    def issue_load(self):
        if self.nissued >= len(self.loads):
            return
        i = self.nissued
        self.nissued += 1
        name, src, kc, n = self.loads[i]
        sl = i % NSLOT
        dst = self.slots[sl][:, 0:kc * n].rearrange("p (k n) -> p k n", k=kc)
        self.dma("pool", dst, src, "wsem%d" % sl, (), [("ws", sl)])

    def getw(self, name):
        i = self.nload
        self.nload += 1
        lname, src, kc, n = self.loads[i]
        assert lname == name, (lname, name)
        sl = i % NSLOT
        view = self.slots[sl][:, 0:kc * n].rearrange("p (k n) -> p k n", k=kc)
        return view, ("ws", sl)

    def donew(self):
        self.issue_load()

    def setup(self):
        d = self.d
        K = []
        for nm, dst in [("cols", self.cols), ("rows", self.rows), ("cons", self.cons)]:
            self.dma("sp", dst, d[nm], "setup", (), [nm])
            K.append(nm)
        self.dma("pool", self.bda, d["bda"], "setup2", (), ["bda"])
        self.dma("pool", self.bdx, d["bdx"], "setup2", (), ["bdx"])
        self.dma("pool", self.wif, d["wif"].rearrange("(k p) n -> p k n", p=128), "setup2", (), ["wif"])
        self.mset(self.misc[:, 0:1], EPS, ["misc"])
        self.mset(self.misc[:, 1:2], 1.0, ["misc"])
        self.mset(self.misc[:, 2:3], 0.0, ["misc"])
        self.mset(self.misc[:, 3:4], float(np.pi / 2), ["misc"])
        self.cp(self.consb, self.cons, ["cons"], ["consb"])
        for j in range(4):
            src = self.con("mprev") if j % 2 == 0 else self.con("tri")
            self.ts(self.mask4[:, j * 128:(j + 1) * 128], src, 30000.0, -30000.0, ALU.mult, ALU.add, ["consb"], ["mask4"])
        o, w = COLS["rglam"]
        t = self.sm[0]
        self.act(t[:, 0:4], self.cols[:, o:o + 4], AF.Exp, ["cols"], ["sm0"], scale=-1.0)
        self.act(t[:, 4:8], t[:, 0:4], AF.Ln, ["sm0", "misc"], ["sm0"], bias=self.misc[:, 1:2])
        self.ts(self.cA[:, 0:4], t[:, 4:8], -8.0, None, ALU.mult, None, ["sm0"], ["cA"])
        self.ts(self.cA[:, 4:8], t[:, 4:8], -16.0, None, ALU.mult, None, ["sm0"], ["cA"])
        bino, _ = COLS["bin"]
        for i in range(12):
            if i < 4:
                wl = self.cols[:, COLS["rgcw"][0] + i * 4 + 3:COLS["rgcw"][0] + i * 4 + 4]
                cb = self.col("rgcb", i)
                bi = self.cols[:, bino + i:bino + i + 1]
            else:
                ci = i - 4
                wl = self.cols[:, COLS["mlcw"][0] + ci * 4 + 3:COLS["mlcw"][0] + ci * 4 + 4]
                cb = self.col("mlcb", ci)
                bi = self.cols[:, bino + 8 + ci:bino + 9 + ci]
            self.stt(self.cb2[:, i:i + 1], bi, wl, cb, ALU.mult, ALU.add, ["cols"], ["cb2"])
        o, w = ROWS["snk"]
        self.act(self.esink, self.rows[:, o:o + 16], AF.Exp, ["rows"], ["esink"])
        for buf, key in [(self.rg_halo, "rghalo"), (self.rg_h, "rgh"), (self.ml_halo, "mlhalo"),
                         (self.ff_halo[0], "ffhalo0"),
                         (self.ff_halo[1], "ffhalo1"), (self.vbuf, "vbuf"),
]:
            self.mset(buf, 0.0, [key])
        for g in range(4):
            self.mset(self.kbuf[:, 2 * g:2 * g + 2, :], 0.0, [("kbuf", g)])
        for hd in range(4):
            self.mset(self.Cf[:, hd, :], 0.0, [("Cf", hd)])
            self.mset(self.Cb[:, hd, :], 0.0, [("Cb", hd)])
        for q_ in range(NT):
            self.mset(self.vaug[:, q_, :, 128:129], 1.0, [("vaug", q_)])
        self.mset(self.vbuf[:, :, :, 64:65], 1.0, ["vbuf"])

    def rmsnorm(self, xb, xk, gname):
        for kc in range(8):
            self.act(self.xsq[:, kc, :], xb[:, kc, :], AF.Square, [(xk, kc)], ["hid"])
        ps, pk = self.bank()
        for kc in range(8):
            self.mm(ps, self.con("ones"), self.xsq[:, kc, :], kc == 0, kc == 7, ["hid", "consb"], [pk])
        self.act(self.rstd, ps, AF.Ln, [pk, "misc"], ["rstd"], bias=self.misc[:, 0:1], scale=1.0 / D)
        self.act(self.rstd, self.rstd, AF.Exp, ["rstd"], ["rstd"], scale=-0.5)
        for kc in range(8):
            self.stt(self.h[:, kc, :], xb[:, kc, :], self.col(gname, kc), self.rstd, ALU.mult, ALU.mult,
                     [(xk, kc), "rstd", "cols"], ["h"])

    def conv(self, ps, pk, halo, hk, wname, widx, ktaps, bias_ap, pre_bias=None, bias2=None):
        hl = ktaps - 1
        buf, bk = self.rotbuf("cvin", self.cvin)
        acc, ak = self.rotbuf("cvacc", self.cvacc)
        self.cp(buf[:, 0:hl], halo, [hk], [bk])
        if pre_bias is not None:
            self.act(buf[:, hl:hl + TB], ps, AF.Identity, [pk, "cols"], [bk], bias=pre_bias)
        else:
            self.cp(buf[:, hl:hl + TB], ps, [pk], [bk], eng="act")
        self.cp(halo, buf[:, TB:TB + hl], [bk], [hk])
        wo, _ = COLS[wname]
        wc = lambda j: self.cols[:, wo + widx * ktaps + j:wo + widx * ktaps + j + 1]
        self.act(acc[:, 0:TB], ps, AF.Identity, [pk, "cols", "cb2"], [ak], bias=(bias2 if bias2 is not None else bias_ap),
                 scale=wc(ktaps - 1))
        for j in range(ktaps - 2, -1, -1):
            self.stt(acc[:, 0:TB], buf[:, j:j + TB], wc(j), acc[:, 0:TB], ALU.mult, ALU.add, [bk, ak, "cols"], [ak])
        return acc, ak

    def proj(self, wv, wk, cols0, actb, ak, nk):
        ps, pk = self.bank()
        for kc in range(nk):
            self.mm(ps, wv[:, kc, cols0:cols0 + 128], actb[:, kc, :], kc == 0, kc == nk - 1,
                    [wk] + (list(ak) if isinstance(ak, list) else [ak]), [pk])
        return ps, pk

    def mixer0(self, blk, xb, xk):
        self.rmsnorm(xb, xk, "nmix0")
        bino, _ = COLS["bin"]
        for gi, dst, dk in [(2, self.qfm, 0), (3, self.kfm, 4)]:
            wv, wk = self.getw("w_in%d" % gi)
            for c in range(4):
                ci = (gi - 2) * 4 + c
                ps, pk = self.proj(wv, wk, c * 128, self.h, "h", 8)
                acc, ak = self.conv(ps, pk, self.ml_halo[:, ci, :], "mlhalo", "mlcw", ci, 4,
                                    self.col("mlcb", ci), pre_bias=self.cols[:, bino + 8 + ci:bino + 9 + ci],
                                    bias2=self.cb2[:, 4 + ci:5 + ci])
                self.act(dst[:, c, :], acc[:, 0:TB], AF.Silu, [ak], [("qk8", dk + c)])
            self.donew()
        io, _ = ROWS["bif"]
        for tt_ in range(NT):
            ps, pk = self.bank()
            for kc in range(8):
                self.mm(ps[:, 0:8], self.h[:, kc, tt_ * 128:(tt_ + 1) * 128], self.wif[:, kc, :], kc == 0, kc == 7,
                        ["h", "wif"], [pk])
            self.tt(self.ifs[:, tt_, :], ps[:, 0:8], self.rows[:, io:io + 8], ALU.add, [pk, "rows"], [("ifs", tt_)])
        wv4, wk4 = self.getw("w_in4")
        wv5, wk5 = self.getw("w_in5")
        bo, _ = ROWS["bvo"]
        mo, _ = ROWS["mln"]
        for tt_ in range(NT):
            ps, pk = self.bank()
            for kc in range(8):
                self.mm(ps, self.h[:, kc, tt_ * 128:(tt_ + 1) * 128], wv4[:, kc, :], kc == 0, kc == 7, ["h", wk4], [pk])
            self.tt(self.vaug[:, tt_, :, 0:128], ps.rearrange("p (h e) -> p h e", h=4),
                    self.rows[:, bo:bo + 512].rearrange("p (h e) -> p h e", h=4), ALU.add,
                    [pk, "rows"], [("vaug", tt_)])
            ps, pk = self.bank()
            for kc in range(8):
                self.mm(ps, self.h[:, kc, tt_ * 128:(tt_ + 1) * 128], wv5[:, kc, :], kc == 0, kc == 7, ["h", wk5], [pk])
            t1, t1k = self.rotbuf("tf", self.tf)
            self.tt(t1[:, 0:512], ps, self.rows[:, bo + 512:bo + 1024], ALU.add, [pk, "rows"], [t1k])
            self.act(t1[:, 0:512], t1[:, 0:512], AF.Sigmoid, [t1k], [t1k])
            self.tt(self.og[:, tt_, :], t1[:, 0:512], self.rows[:, mo:mo + 512], ALU.mult, [t1k, "rows"], [("og", tt_)])
        self.donew()
        self.donew()
        cq = float(128.0 ** -0.5)
        for tt_ in range(NT):
            tsl = slice(tt_ * 128, (tt_ + 1) * 128)
            sm, smk = self.rotbuf("sm", self.sm)
            self.act(sm[:, 0:4], self.ifs[:, tt_, 4:8], AF.Exp, [("ifs", tt_)], [smk], scale=-1.0)
            self.act(sm[:, 4:8], sm[:, 0:4], AF.Ln, [smk, "misc"], [smk], bias=self.misc[:, 1:2])
            self.ts(sm[:, 8:12], sm[:, 4:8], -1.0, None, ALU.mult, None, [smk], [smk])
            ra, rak = self.rotbuf("rhsall", self.rhsall)
            for hd in range(4):
                self.ts(ra[:, hd, :], self.con("tri", bf=False), sm[:, 8 + hd:9 + hd], None, ALU.mult, None,
                        [smk, "cons"], [rak])
            psb, pbk = self.bank()
            self.mm(psb[:, 0:4], self.con("tri", bf=False), sm[:, 8:12], True, True, ["cons", smk], [pbk])
            pse, pek = self.bank()
            self.mm(pse, self.con("ones", bf=False), ra.rearrange("p h t -> p (h t)"), True, True, ["cons", rak], [pek])
            Eb, ebk = self.rotbuf("Eb", self.Eb)
            self.act(Eb.rearrange("p h t -> p (h t)"), pse, AF.Exp, [pek], [ebk])
            self.tt(sm[:, 12:16], self.ifs[:, tt_, 0:4], psb[:, 0:4], ALU.subtract, [("ifs", tt_), pbk], [smk])
            self.act(sm[:, 16:20], sm[:, 12:16], AF.Exp, [smk], [smk])
            self.tt(sm[:, 20:24], sm[:, 16:20], Eb[:, :, 127], ALU.mult, [smk, ebk], [smk])
            hm, hmk = self.rotbuf("hm", self.hm)
            for hd in range(4):
                qt, qtk = self.rotbuf("tb", self.tb)
                self.stt(qt[:, 0:128], self.qfm[:, hd, tsl], cq, Eb[:, hd, :], ALU.mult, ALU.mult, [("qk8", hd), ebk], [qtk])
                pss, psk = self.bank()
                self.mm(pss[:, 0:128], self.kfm[:, hd, tsl], qt[:, 0:128], True, True, [("qk8", 4 + hd), qtk], [psk])
                sq, sqk = self.rotbuf("tb", self.tb)
                self.stt(sq[:, 0:128], pss[:, 0:128], sm[:, 16 + hd:17 + hd], self.con("tri", bf=False),
                         ALU.mult, ALU.mult, [psk, smk, "cons"], [sqk])
                psn, pnk = self.bank()
                self.mm(psn[:, 0:129], sq[:, 0:128], self.vaug[:, tt_, hd, :], True, False, [sqk, ("vaug", tt_)], [pnk])
                self.mm(psn[:, 0:129], qt[:, 0:128], self.Cb[:, hd, :], False, True, [qtk, ("Cb", hd)], [pnk])
                self.ts(sm[:, 40 + hd:41 + hd], psn[:, 128:129], 1.0, None, ALU.max, None, [pnk], [(smk, hd)])
                self.stt(sm[:, 24 + hd:25 + hd], psn[:, 128:129], -1.0, sm[:, 40 + hd:41 + hd], ALU.mult, ALU.max, [pnk, (smk, hd)], [(smk, hd)])
                self.rcp(sm[:, 28 + hd:29 + hd], sm[:, 24 + hd:25 + hd], [(smk, hd)], [(smk, hd)])
                self.ts(hm[:, hd, :], psn[:, 0:128], sm[:, 28 + hd:29 + hd], None, ALU.mult, None, [pnk, (smk, hd)], [(hmk, hd)])
                pst, ptk = self.bank()
                pstb = pst.bitcast(BF16)
                self.tr(pstb[:, 0:128], self.kfm[:, hd, tsl], self.con("ident"), [("qk8", 4 + hd), "consb"], [ptk])
                kw, kwk = self.rotbuf("tb", self.tb)
                self.act(kw[:, 0:128], pstb[:, 0:128], AF.Identity, [ptk, smk], [kwk], scale=sm[:, 20 + hd:21 + hd])
                psc, pck = self.bank()
                self.mm(psc[:, 0:129], kw[:, 0:128], self.vaug[:, tt_, hd, :], True, True, [kwk, ("vaug", tt_)], [pck])
                self.stt(self.Cf[:, hd, :], self.Cf[:, hd, :], Eb[:, hd, 127:128], psc[:, 0:129], ALU.mult, ALU.add,
                         [("Cf", hd), ebk, pck], [("Cf", hd)])
                self.cp(self.Cb[:, hd, :], self.Cf[:, hd, :], [("Cf", hd)], [("Cb", hd)], eng="act")
            if blk == 0 and tt_ == 0:
                self.dump("sm", sm, [smk] + [(smk, q_) for q_ in range(4)])
                self.dump("Eb", Eb, [ebk])
                self.dump("ra", ra, [rak])
                self.dump("hm", hm, [(hmk, q_) for q_ in range(4)])
            jb, jk = self.rotbuf("tf", self.tf)
            junk = jb[:, 0:512].rearrange("p (h e) -> p h e", h=4)
            self.act(junk, hm, AF.Square, [(hmk, q_) for q_ in range(4)], [jk])
            self.s.op("dve", (lambda o_, i_: (lambda e: e.tensor_reduce(out=o_, in_=i_, axis=mybir.AxisListType.X, op=ALU.add)))(
                sm[:, 32:36], junk), [jk], [smk], dur=0.65)
            self.act(sm[:, 36:40], sm[:, 32:36], AF.Ln, [smk, "misc"], [smk], bias=self.misc[:, 0:1], scale=1.0 / 128)
            self.act(sm[:, 36:40], sm[:, 36:40], AF.Exp, [smk], [smk], scale=-0.5)
            yt, ytk = self.rotbuf("ytok", self.ytok)
            for hd in range(4):
                self.stt(yt[:, hd * 128:(hd + 1) * 128], hm[:, hd, :], sm[:, 36 + hd:37 + hd],
                         self.og[:, tt_, hd * 128:(hd + 1) * 128], ALU.mult, ALU.mult, [(hmk, hd), smk, ("og", tt_)], [ytk])
            psy, pyk = self.bank()
            psyb = psy.bitcast(BF16)
            for hd in range(4):
                self.tr(psyb[:, hd * 128:(hd + 1) * 128], yt[:, hd * 128:(hd + 1) * 128], self.con("ident"),
                        [ytk, "consb"], [pyk])
            self.cp(self.yfm[:, 4:8, tsl], psyb[:, 0:512].rearrange("p (h t) -> p h t", h=4), [pyk], [("yfm", 4 + q_) for q_ in range(4)], eng="act")
        w0, k0 = self.getw("w_in0")
        w1, k1 = self.getw("w_in1")
        for c in range(4):
            ps, pk = self.proj(w0, k0, c * 128, self.h, "h", 8)
            xc, xck = self.conv(ps, pk, self.rg_halo[:, c, :], "rghalo", "rgcw", c, 4,
                                self.col("rgcb", c), pre_bias=self.cols[:, bino + c:bino + c + 1], bias2=self.cb2[:, c:c + 1])
            xcb, xcbk = self.rotbuf("tb", self.tb)
            self.cp(xcb[:, 0:TB], xc[:, 0:TB], [xck], [xcbk], eng="act")
            psr, prk = self.bank()
            self.mm(psr, self.bda[:, c, :], xcb[:, 0:TB], True, True, ["bda", xcbk], [prk])
            psi, pik = self.bank()
            self.mm(psi, self.bdx[:, c, :], xcb[:, 0:TB], True, True, ["bdx", xcbk], [pik])
            rr, rk = self.rotbuf("tf", self.tf)
            ii, ik = self.rotbuf("tf", self.tf)
            self.act(rr[:, 0:TB], psr, AF.Sigmoid, [prk, "cols"], [rk], bias=self.col("rgba", c))
            self.act(ii[:, 0:TB], psi, AF.Sigmoid, [pik, "cols"], [ik], bias=self.col("rgbx", c))
            aa, akk = self.rotbuf("tf", self.tf)
            a2, a2k = self.rotbuf("tf", self.tf)
            self.act(aa[:, 0:TB], rr[:, 0:TB], AF.Exp, [rk, "cA"], [akk], scale=self.cA[:, c:c + 1])
            self.act(a2[:, 0:TB], rr[:, 0:TB], AF.Exp, [rk, "cA"], [a2k], scale=self.cA[:, 4 + c:5 + c])
            self.act(a2[:, 0:TB], a2[:, 0:TB], AF.Ln, [a2k, "misc"], [a2k], bias=self.misc[:, 1:2], scale=-1.0)
            self.act(a2[:, 0:TB], a2[:, 0:TB], AF.Exp, [a2k], [a2k], scale=0.5)
            self.tt(ii[:, 0:TB], ii[:, 0:TB], xc[:, 0:TB], ALU.mult, [ik, xck], [ik])
            self.tt(ii[:, 0:TB], ii[:, 0:TB], a2[:, 0:TB], ALU.mult, [ik, a2k], [ik])
            hs, hk = self.rotbuf("tf", self.tf)
            self.s.op("dve", (lambda o_, a_, u_, i_: (lambda e: e.tensor_tensor_scan(
                out=o_, data0=a_, data1=u_, initial=i_, op0=ALU.mult, op1=ALU.add)))(
                hs[:, 0:TB], aa[:, 0:TB], ii[:, 0:TB], self.rg_h[:, c:c + 1]), [akk, ik, "rgh"], [hk], dur=1.2)
            self.cp(self.rg_h[:, c:c + 1], hs[:, TB - 1:TB], [hk], ["rgh"])
            psg, pgk = self.proj(w1, k1, c * 128, self.h, "h", 8)
            gg, ggk = self.rotbuf("tf", self.tf)
            self.act(gg[:, 0:TB], psg, AF.Gelu_apprx_tanh, [pgk, "cols"], [ggk],
                     bias=self.cols[:, bino + 4 + c:bino + 5 + c])
            self.tt(self.yfm[:, c, :], hs[:, 0:TB], gg[:, 0:TB], ALU.mult, [hk, ggk], [("yfm", c)])
        self.donew()
        self.donew()
        if blk == 0:
            self.dump("yfm", self.yfm, [("yfm", q_) for q_ in range(8)])
            self.dump("qk8", self.qk8, [("qk8", q_) for q_ in range(8)])
            self.dump("vaug", self.vaug, [("vaug", q_) for q_ in range(NT)])
            self.dump("og", self.og, [("og", q_) for q_ in range(NT)])
            self.dump("ifs", self.ifs, [("ifs", q_) for q_ in range(NT)])
            self.dump("Cf", self.Cf, [("Cf", q_) for q_ in range(4)])
        for g in range(2):
            wv, wk = self.getw("hyout%d" % g)
            for m in range(4):
                ps, pk = self.proj(wv, wk, m * 128, self.yfm, [("yfm", q_) for q_ in range(8)], 8)
                mm_ = g * 4 + m
                self.tt(xb[:, mm_, :], xb[:, mm_, :], ps, ALU.add, [(xk, mm_), pk], [(xk, mm_)])
            self.donew()

    def ffn(self, l, xb, xk):
        self.rmsnorm(xb, xk, "nffn%d" % l)
        for g in range(6):
            wg, wgk = self.getw("upg%d_%d" % (l, g))
            wu, wuk = self.getw("upu%d_%d" % (l, g))
            for m in range(4):
                j = g * 4 + m
                psg, pgk = self.proj(wg, wgk, m * 128, self.h, "h", 8)
                psu, puk = self.proj(wu, wuk, m * 128, self.h, "h", 8)
                acc, ak = self.conv(psg, pgk, self.ff_halo[l][:, j, :], "ffhalo%d" % l, "ffcw%d" % l, j, 3,
                                    self.col("ffcb%d" % l, j))
                ge, gk = self.rotbuf("tf", self.tf)
                self.act(ge[:, 0:TB], acc[:, 0:TB], AF.Gelu_apprx_tanh, [ak], [gk])
                self.tt(self.hid[:, j, :], ge[:, 0:TB], psu, ALU.mult, [gk, puk], ["hid"])
            self.donew()
            self.donew()
        for m in range(8):
            wv, wk = self.getw("dn%d_%d" % (l, m))
            ps, pk = self.bank()
            for j in range(24):
                self.mm(ps, wv[:, j, :], self.hid[:, j, :], j == 0, j == 23, [wk, "hid"], [pk])
            self.tt(xb[:, m, :], xb[:, m, :], ps, ALU.add, [(xk, m), pk], [(xk, m)])
            self.donew()

    def ple(self, l, blk, xb, xk):
        self.rmsnorm(xb, xk, "nple%d" % l)
        for g in range(2):
            wp, wpk = self.getw("pp%d_%d" % (l, g))
            wv, wk = self.getw("pg%d_%d" % (l, g))
            for m4 in range(4):
                m = g * 4 + m4
                ps, pk = self.proj(wv, wk, m4 * 128, self.h, "h", 8)
                gt, gk = self.rotbuf("tf", self.tf)
                self.act(gt[:, 0:TB], ps, AF.Sigmoid, [pk], [gk])
                ps2, pk2 = self.bank()
                for kc in range(2):
                    self.mm(ps2, wp[:, kc, m4 * 128:(m4 + 1) * 128], self.pblk[l][:, kc, :], kc == 0, kc == 1,
                            [wpk, ("pblk", l)], [pk2])
                self.tt(gt[:, 0:TB], gt[:, 0:TB], ps2, ALU.mult, [gk, pk2], [gk])
                self.tt(xb[:, m, :], xb[:, m, :], gt[:, 0:TB], ALU.add, [(xk, m), gk], [(xk, m)])
            self.donew()
            self.donew()

    def rope_tables(self, blk):
        t0 = blk * TB
        self.dma("sp", self.posi, self.d["pos"][0, t0:t0 + TB].partition_broadcast(128), "possem", (), ["posi"])
        TWO_PI = float(2 * np.pi)
        for which, dst, dk in [(0, self.sinb, "sin"), (1, self.cosb, "cos")]:
            ang, ak = self.rotbuf("tf", self.tf)
            nn, nk = self.rotbuf("tf", self.tf)
            ni = nn.bitcast(I32)
            self.cp(ang[:, 0:TB], self.posi, ["posi"], [ak])
            if which == 0:
                self.ts(ang[:, 0:TB], ang[:, 0:TB], self.col("invf"), None, ALU.mult, None, [ak, "cols"], [ak])
            else:
                self.ts(ang[:, 0:TB], ang[:, 0:TB], self.col("invf"), float(np.pi / 2), ALU.mult, ALU.add, [ak, "cols"], [ak])
            self.ts(ni[:, 0:TB], ang[:, 0:TB], 1.0 / TWO_PI, None, ALU.mult, None, [ak], [nk])
            red, rk = self.rotbuf("tf", self.tf)
            self.cp(red[:, 0:TB], ni[:, 0:TB], [nk], [rk])
            self.stt(red[:, 0:TB], red[:, 0:TB], -TWO_PI, ang[:, 0:TB], ALU.mult, ALU.add, [rk, ak], [rk])
            self.ts(red[:, 0:TB], red[:, 0:TB], float(np.pi), float(-np.pi), ALU.min, ALU.max, [rk], [rk])
            self.act(dst, red[:, 0:TB], AF.Sin, [rk], [dk])

    def qk_norm_rope(self, ps, pk, gname, dst, dk):
        qsq, qk_ = self.rotbuf("tb", self.tb)
        self.act(qsq[:, 0:TB], ps, AF.Square, [pk], [qk_])
        pss, psk = self.bank()
        self.mm(pss, self.con("hones"), qsq[:, 0:TB], True, True, ["consb", qk_], [psk])
        sd, sk = self.rotbuf("tf", self.tf)
        self.act(sd[:, 0:TB], pss, AF.Ln, [psk, "misc"], [sk], bias=self.misc[:, 0:1], scale=1.0 / 64)
        self.act(sd[:, 0:TB], sd[:, 0:TB], AF.Exp, [sk], [sk], scale=-0.5)
        qn, qnk = self.rotbuf("tb", self.tb)
        self.stt(qn[:, 0:TB], ps, self.col(gname), sd[:, 0:TB], ALU.mult, ALU.mult, [pk, sk, "cols"], [qnk])
        psr, prk = self.bank()
        self.mm(psr, self.con("rt"), qn[:, 0:TB], True, True, ["consb", qnk], [prk])
        t1, t1k = self.rotbuf("tf", self.tf)
        t2, t2k = self.rotbuf("tf", self.tf)
        self.tt(t1[:, 0:TB], qn[:, 0:TB], self.cosb, ALU.mult, [qnk, "cos"], [t1k], eng="pool")
        self.tt(t2[:, 0:TB], psr, self.sinb, ALU.mult, [prk, "sin"], [t2k])
        if isinstance(dst, tuple):
            d0, d1 = dst
            self.tt(d0[0:64], t1[0:64, 0:TB], t2[0:64, 0:TB], ALU.add, [t1k, t2k], [dk], eng="pool")
            self.tt(d1[64:128], t1[64:128, 0:TB], t2[64:128, 0:TB], ALU.add, [t1k, t2k], [dk], eng="pool")
        else:
            self.tt(dst, t1[:, 0:TB], t2[:, 0:TB], ALU.add, [t1k, t2k], [dk], eng="pool")

    def mixer1(self, blk, xb, xk):
        self.rmsnorm(xb, xk, "nmix1")
        for g in range(2):
            wv, wk = self.getw("wq%d" % g)
            for m in range(4):
                c = g * 4 + m
                ps, pk = self.proj(wv, wk, m * 128, self.h, "h", 8)
                self.qk_norm_rope(ps, pk, "qn", self.qrope[:, c, :], ("qk8", c))
            self.donew()
        wv, wk = self.getw("wkd")
        for g in range(4):
            ps, pk = self.proj(wv, wk, g * 128, self.h, "h", 8)
            self.qk_norm_rope(ps, pk, "kn", (self.kbuf[:, 2 * g, 128:128 + TB], self.kbuf[:, 2 * g + 1, 128:128 + TB]), ("kbuf", g))
        self.donew()
        wv, wk = self.getw("wv")
        for tt_ in range(NT):
            ps, pk = self.bank()
            for kc in range(8):
                self.mm(ps[:, 0:256], self.h[:, kc, tt_ * 128:(tt_ + 1) * 128], wv[:, kc, :], kc == 0, kc == 7, ["h", wk], [pk])
            self.cp(self.vbuf[:, 1 + tt_, :, 0:64], ps[:, 0:256].rearrange("p (g e) -> p g e", g=4), [pk], ["vbuf"], eng="act")
        self.donew()
        so, _ = ROWS["snk"]
        for tt_ in range(NT):
            first = (blk == 0 and tt_ == 0)
            yt, ytk = self.rotbuf("ytok", self.ytok)
            for cp_ in range(4):
                g = cp_
                pes = []
                for j in range(2):
                    pss, psk = self.bank()
                    for ci in range(2):
                        c = 2 * cp_ + ci
                        q = self.qrope[:, c, tt_ * 128:(tt_ + 1) * 128]
                        for half in range(2):
                            o_ = pss[:, ci * 256 + half * 128:ci * 256 + half * 128 + 128]
                            kcol = half * 128 + tt_ * 128
                            self.mm(o_, self.kbuf[:, 2 * g + j, kcol:kcol + 128], q, True, False, [("kbuf", g), ("qk8", c)], [psk])
                            self.mm(o_, self.con("ident"), self.mask4[:, half * 128:(half + 1) * 128], False, True,
                                    ["consb", "mask4"], [psk])
                    pe_, pek = self.rotbuf("tb", self.tb)
                    self.act(pe_[:, 0:512], pss, AF.Exp, [psk], [pek], scale=0.125)
                    pes.append((pe_, pek))
                pso, pok = self.bank()
                for ci in range(2):
                    for j in range(2):
                        idx = ci * 2 + j
                        pe_, pek = pes[j]
                        o_ = pso[:, idx * 65:(idx + 1) * 65]
                        if not first:
                            self.mm(o_, pe_[:, ci * 256:ci * 256 + 128], self.vbuf[:, tt_, g, :], True, False, [pek, "vbuf"], [pok])
                        self.mm(o_, pe_[:, ci * 256 + 128:ci * 256 + 256], self.vbuf[:, tt_ + 1, g, :], first, True, [pek, "vbuf"], [pok])
                sm, smk = self.rotbuf("sm", self.sm)
                self.tt(sm[:, 0:4], pso[:, 0:260].rearrange("p (j e) -> p j e", j=4)[:, :, 64],
                        self.esink[:, 4 * cp_:4 * cp_ + 4], ALU.add, [pok, "esink"], [smk])
                self.rcp(sm[:, 4:8], sm[:, 0:4], [smk], [smk])
                for idx in range(4):
                    hq = 4 * cp_ + idx
                    self.ts(yt[:, hq * 64:(hq + 1) * 64], pso[:, idx * 65:idx * 65 + 64], sm[:, 4 + idx:5 + idx], None, ALU.mult, None,
                            [pok, smk], [ytk])
            psy, pyk = self.bank()
            psyb = psy.bitcast(BF16)
            for c in range(8):
                self.tr(psyb[:, c * 128:(c + 1) * 128], yt[:, c * 128:(c + 1) * 128], self.con("ident"), [ytk, "consb"], [pyk])
            self.cp(self.yfm[:, :, tt_ * 128:(tt_ + 1) * 128], psyb.rearrange("p (c t) -> p c t", c=8), [pyk], [("yfm", q_) for q_ in range(8)], eng="act")
        for g in range(4):
            self.cp(self.kbuf[:, 2 * g:2 * g + 2, 0:128], self.kbuf[:, 2 * g:2 * g + 2, TB:TB + 128], [("kbuf", g)], [("kbuf", g)])
        self.cp(self.vbuf[:, 0, :, :], self.vbuf[:, NT, :, :], ["vbuf"], ["vbuf"])
        for g in range(2):
            wv, wk = self.getw("atout%d" % g)
            for m in range(4):
                ps, pk = self.proj(wv, wk, m * 128, self.yfm, [("yfm", q_) for q_ in range(8)], 8)
                mm_ = g * 4 + m
                self.tt(xb[:, mm_, :], xb[:, mm_, :], ps, ALU.add, [(xk, mm_), pk], [(xk, mm_)])
            self.donew()

    def build(self):
        d = self.d
        self.plan_loads()
        self.setup()
        for _ in range(NSLOT):
            self.issue_load()
        xT = d["xT"].rearrange("(c p) t -> p c t", p=128)
        oT = d["outT"].rearrange("(c p) t -> p c t", p=128)
        ns = self.nstage
        outkeys = []
        def load_x(b_):
            self.dma("sp", self.x[b_ % 2], xT[:, :, b_ * TB:(b_ + 1) * TB], "xin%d" % (b_ % 2), (), [(("x", b_ % 2), m_) for m_ in range(8)])

        def load_p(b_):
            for l_ in range(2):
                if ns >= 3 + 3 * l_:
                    self.dma("pool", self.pblk[l_], d["pT"][l_].rearrange("(k p) t -> p k t", p=128)[:, :, b_ * TB:(b_ + 1) * TB],
                             "psem%d" % l_, (), [("pblk", l_)])

        load_x(0)
        for blk in range(self.nblk):
            par = blk % 2
            t0 = blk * TB
            xb, xk = self.x[par], ("x", par)
            if blk + 1 < self.nblk:
                load_x(blk + 1)
            load_p(blk)
            if ns >= 4 and "mix1" not in self.skip:
                self.rope_tables(blk)
            for l in range(2):
                if l == 0 and ns >= 1 and "mix0" not in self.skip:
                    self.mixer0(blk, xb, xk)
                if l == 1 and ns >= 4 and "mix1" not in self.skip:
                    self.mixer1(blk, xb, xk)
                if ns >= 2 + 3 * l and ("ffn%d" % l) not in self.skip:
                    self.ffn(l, xb, xk)
                if ns >= 3 + 3 * l and ("ple%d" % l) not in self.skip:
                    self.ple(l, blk, xb, xk)
            self.dma("sp", oT[:, :, t0:t0 + TB], xb, "xout%d" % par, [(xk, m_) for m_ in range(8)], [("out", blk)])
            outkeys.append(("out", blk))
        assert self.nload == len(self.loads), (self.nload, len(self.loads))
        self.s.final_wait("sp", outkeys + self.dbgkeys)
        nc = self.nc
        sch = self.s
        sch.finalize(reorder=self.reorder, keep=self.keep)
        with nc.Block() as block:
            @block.tensor
            def _(e):
                sch.replay("pe", e)

            @block.scalar
            def _(e):
                sch.replay("act", e)

            @block.vector
            def _(e):
                sch.replay("dve", e)

            @block.gpsimd
            def _(e):
                sch.replay("pool", e)

            @block.sync
            def _(e):
                sch.replay("sp", e)
        return nc


def kernel(**inputs):
    x = np.asarray(inputs["x"], np.float32)
    p = np.asarray(inputs["p"], np.float32)
    pos = np.asarray(inputs["positions"], np.int32)
    shared = host_tables(inputs)
    nb = x.shape[0]
    in_maps = []
    for b in range(nb):
        m = dict(shared)
        m["xT"] = np.ascontiguousarray(x[b].T)
        m["pT"] = np.ascontiguousarray(p[:, b].transpose(0, 2, 1))
        m["pos"] = np.ascontiguousarray(pos[b].reshape(1, S))
        in_maps.append(m)
    prog = Prog(nblk=8, nstage=6)
    nc = prog.build()
    res = run_bass_kernel_spmd(nc, in_maps, core_ids=list(range(nb)))
    out = np.stack([np.ascontiguousarray(r["outT"].T) for r in res.results], axis=0)
    return out.astype(np.float32)
```

```python
import numpy as np
import concourse.bass as bass
import concourse.mybir as mybir
from concourse.bass_utils import run_bass_kernel_spmd

F32 = mybir.dt.float32
BF16 = mybir.dt.bfloat16
I32 = mybir.dt.int32
AF = mybir.ActivationFunctionType
ALU = mybir.AluOpType

S = 4096
D = 1024
TB = 512
NT = 4
EPS = 1e-6
NSLOT = 5
SAME_ENG_SYNC = True
ENGS = ("pe", "act", "dve", "pool", "sp")
ACT_SET = {AF.Exp: "le", AF.Ln: "le", AF.Sigmoid: "sg", AF.Gelu_apprx_tanh: "ge", AF.Silu: "si", AF.Sqrt: "sq", AF.Sin: "sn"}


class Sched:
    GROUP_SEMS = ("setup", "setup2")
    LAT = 0.3
    PRIO = False
    SAME_LAT = 0.12
    ACT_BIAS = 60

    def __init__(self, nc):
        self.nc = nc
        self.ops = []
        self.sem = {}
        for e in ENGS:
            self._sem(e)

    def _sem(self, name):
        if name not in self.sem:
            self.sem[name] = self.nc.alloc_semaphore("s_" + name)
        return self.sem[name]

    def op(self, e, fn, r=(), w=(), dur=0.5):
        self.ops.append(dict(eng=e, fn=fn, r=list(r), w=list(w), kind="op", dur=dur))

    def dma(self, q, fn, sem, r=(), w=(), nbytes=0):
        self._sem(sem)
        self.ops.append(dict(eng=q, fn=fn, r=list(r), w=list(w), kind="dma", sem=sem, nbytes=nbytes, dur=0.0))

    def final_wait(self, e, keys):
        self.ops.append(dict(eng=e, fn=None, r=list(keys), w=[], kind="wait", dur=0.0))

    def finalize(self, reorder=True, keep=()):
        import heapq
        ops = self.ops
        n = len(ops)
        lastw, readers = {}, {}
        preds = [None] * n
        for i, o in enumerate(ops):
            ps = set()
            for k in o["r"]:
                if k in lastw:
                    ps.add(lastw[k])
            for k in o["w"]:
                if k in lastw:
                    ps.add(lastw[k])
                ps |= readers.get(k, set())
            ps.discard(i)
            preds[i] = ps
            for k in o["w"]:
                lastw[k] = i
                readers[k] = set()
            for k in o["r"]:
                readers.setdefault(k, set()).add(i)
        openg = {}
        for i, o in enumerate(ops):
            g = o.get("grp")
            if g is None:
                continue
            key = tuple(o["w"])
            if g[0] and not g[1]:
                openg[key] = [i]
            elif key in openg:
                openg[key].append(i)
                if g[1]:
                    mem = openg.pop(key)
                    ms = set(mem)
                    for m_ in mem[1:]:
                        preds[mem[0]] |= (preds[m_] - ms)
        succs = [[] for _ in range(n)]
        npend = [0] * n
        for i in range(n):
            npend[i] = len(preds[i])
            for p in preds[i]:
                succs[p].append(i)
        lp = [0.0] * n
        for i in range(n - 1, -1, -1):
            m_ = 0.0
            for sidx in succs[i]:
                if lp[sidx] > m_:
                    m_ = lp[sidx]
            o = ops[i]
            d_ = o["dur"] if o["kind"] == "op" else (2.0 + o.get("nbytes", 0) / 330e3 if o["kind"] == "dma" else 0.0)
            lp[i] = m_ + d_ + 0.2
        PRIO = self.PRIO
        order = {e: [] for e in ENGS}
        if not reorder:
            for i, o in enumerate(ops):
                order[o["eng"]].append(i)
        else:
            finish = [0.0] * n
            rt = [0.0] * n
            self.start = [0.0] * n
            self.why = [None] * n
            self.rtby = [None] * n
            lastop = {e: None for e in ENGS}
            tfree = {e: 0.0 for e in ENGS}
            ready = {e: [] for e in ENGS}
            dma_free = [0.0]
            for i in range(n):
                if npend[i] == 0:
                    heapq.heappush(ready[ops[i]["eng"]], (0.0, i))
            act_set = [None]
            ACT_BIAS = self.ACT_BIAS
            self.n_act_switch = 0
            pe_lock = [None]
            nxt = {e: 0 for e in ENGS}
            plist = {e: [i for i in range(n) if ops[i]["eng"] == e] for e in ENGS}
            done = 0
            while done < n:
                best = None
                for e in ENGS:
                    h = ready[e]
                    if not h:
                        continue
                    te = tfree[e]
                    cand = None
                    if e == "pe" and pe_lock[0] is not None:
                        cand = None
                        for x in h:
                            if x[1] == pe_lock[0]:
                                cand = x
                                break
                        if cand is None:
                            continue
                        st = max(te, cand[0])
                    elif e in keep:
                        want = plist[e][nxt[e]]
                        cand = None
                        for x in h:
                            if x[1] == want:
                                cand = x
                                break
                        if cand is None:
                            continue
                        st = max(te, cand[0])
                    elif h[0][0] <= te:
                        tmp = []
                        while h and h[0][0] <= te:
                            tmp.append(heapq.heappop(h))
                        if e == "act":
                            def _k(x):
                                a_ = ops[x[1]].get("aset")
                                sw = 1 if (a_ is not None and a_ != act_set[0]) else 0
                                return (x[1] + (ACT_BIAS if sw else 0), x[1])
                            tmp.sort(key=_k)
                        elif PRIO:
                            tmp.sort(key=lambda x: (-lp[x[1]], x[1]))
                        else:
                            tmp.sort(key=lambda x: x[1])
                        cand = tmp[0]
                        for x in tmp[1:]:
                            heapq.heappush(h, (x[0], x[1]))
                        heapq.heappush(h, cand)
                        st = te
                    else:
                        cand = h[0]
                        st = cand[0]
                    if best is None or st < best[0] or (st == best[0] and cand[1] < best[2][1]):
                        best = (st, e, cand)
                st, e, cand = best
                h = ready[e]
                h.remove(cand)
                heapq.heapify(h)
                i = cand[1]
                o = ops[i]
                self.start[i] = st
                self.why[i] = ("eng", lastop[e]) if (st > rt[i] + 1e-9 and lastop[e] is not None) else ("dep", self.rtby[i])
                lastop[e] = i
                if e == "act" and o.get("aset") is not None and o["aset"] != act_set[0]:
                    act_set[0] = o["aset"]
                    st += 1.3
                    self.n_act_switch += 1
                if o["kind"] == "dma":
                    issue = 1.2 if e == "pool" else 0.15
                    tfree[e] = st + issue
                    s0 = max(st + issue, dma_free[0])
                    dma_free[0] = s0 + o["nbytes"] / 330e3
                    finish[i] = dma_free[0] + 2.0
                else:
                    finish[i] = st + o["dur"]
                    tfree[e] = finish[i]
                if e == "pe":
                    g = o.get("grp")
                    if g is not None and not g[1]:
                        j = i + 1
                        while not (ops[j]["eng"] == "pe" and ops[j]["w"] == o["w"]):
                            j += 1
                        pe_lock[0] = j
                    else:
                        pe_lock[0] = None
                order[e].append(i)
                nxt[e] += 1
                done += 1
                for sidx in succs[i]:
                    if ops[sidx]["eng"] == e and o["kind"] != "dma":
                        lat = 0.0 if e == "pe" else self.SAME_LAT
                    else:
                        lat = self.LAT
                    v = finish[i] + lat
                    if v > rt[sidx]:
                        rt[sidx] = v
                        self.rtby[sidx] = i
                    npend[sidx] -= 1
                    if npend[sidx] == 0:
                        heapq.heappush(ready[ops[sidx]["eng"]], (rt[sidx], sidx))
            self.est_makespan = max(finish) if n else 0.0
            self.finish = finish
        tok = [None] * n
        cnt = {k: 0 for k in self.sem}
        for e in ENGS:
            for i in order[e]:
                o = ops[i]
                if o["kind"] == "op":
                    cnt[e] += 1
                    tok[i] = (e, cnt[e])
                elif o["kind"] == "dma":
                    cnt[o["sem"]] += 16
                    tok[i] = (o["sem"], cnt[o["sem"]])
        for i, o in enumerate(ops):
            if o["kind"] == "dma" and o["sem"] in self.GROUP_SEMS:
                tok[i] = (o["sem"], cnt[o["sem"]])
        self.streams = {e: [] for e in ENGS}
        for e in ENGS:
            seen = {}
            for i in order[e]:
                o = ops[i]
                need = {}
                for p in preds[i]:
                    t = tok[p]
                    if t is None:
                        continue
                    if need.get(t[0], 0) < t[1]:
                        need[t[0]] = t[1]
                waits = []
                for sname, v in need.items():
                    if sname == e and (e == "pe" or not SAME_ENG_SYNC):
                        continue
                    if seen.get(sname, 0) >= v:
                        continue
                    seen[sname] = v
                    waits.append((sname, v))
                inc = None
                if o["kind"] == "op":
                    inc = (e, 1)
                elif o["kind"] == "dma":
                    inc = (o["sem"], 16)
                self.streams[e].append((waits, o["fn"], inc))

    def replay(self, e, eng):
        for waits, fn, inc in self.streams[e]:
            for s, v in waits:
                eng.wait_ge(self.sem[s], v)
            if fn is None:
                continue
            ins = fn(eng)
            ins.then_inc(self.sem[inc[0]], inc[1])


def _nfree(ap):
    n = 1
    for d in ap.shape[1:]:
        n *= int(d)
    return n


def _fm(v):
    v = np.asarray(v, np.float32)
    c = v.shape[0] // 128
    return np.ascontiguousarray(v.reshape(c, 128).T)


def _taps(w):
    w = np.asarray(w, np.float32)
    k, c = w.shape
    return np.ascontiguousarray(w.T.reshape(c // 128, 128, k).transpose(1, 0, 2).reshape(128, -1))


COLS = {}
_off = 0
for _n, _w in [("nmix0", 8), ("nffn0", 8), ("nple0", 8), ("nmix1", 8), ("nffn1", 8), ("nple1", 8),
               ("bin", 16), ("rgcw", 16), ("rgcb", 4), ("rgba", 4), ("rgbx", 4), ("rglam", 4),
               ("mlcw", 32), ("mlcb", 8), ("ffcw0", 72), ("ffcw1", 72), ("ffcb0", 24), ("ffcb1", 24),
               ("qn", 1), ("kn", 1), ("invf", 1)]:
    COLS[_n] = (_off, _w)
    _off += _w
NCOLS = _off
ROWS = {"bvo": (0, 1024), "bif": (1024, 8), "mln": (1032, 512), "snk": (1544, 16)}
NROWS = 1560
CONS = {"ident": 0, "ones": 128, "tri": 256, "mprev": 384, "rt": 512, "hones": 640}
NCONS = 768


def host_tables(inp):
    cols = np.zeros((128, NCOLS), np.float32)

    def put(name, arr):
        o, w = COLS[name]
        cols[:, o:o + w] = np.asarray(arr, np.float32).reshape(128, w)

    for l in range(2):
        put("nmix%d" % l, _fm(inp["norm_mix"][l]))
        put("nffn%d" % l, _fm(inp["norm_ffn"][l]))
        put("nple%d" % l, _fm(inp["norm_ple"][l]))
        put("ffcw%d" % l, _taps(inp["ff_conv_w"][l]))
        put("ffcb%d" % l, _fm(inp["ff_conv_b"][l]))
    b_in = np.asarray(inp["hy_b_in"][0], np.float32)
    put("bin", _fm(b_in[0:2048]))
    put("rgcw", _taps(inp["rg_conv_w"][0]))
    put("rgcb", _fm(inp["rg_conv_b"][0]))
    put("rgba", _fm(inp["rg_b_a"][0]))
    put("rgbx", _fm(inp["rg_b_x"][0]))
    put("rglam", _fm(inp["rg_lambda"][0]))
    put("mlcw", _taps(inp["ml_conv_w"][0]))
    put("mlcb", _fm(inp["ml_conv_b"][0]))
    put("qn", np.tile(np.asarray(inp["at_q_norm"][0], np.float32), 2))
    put("kn", np.tile(np.asarray(inp["at_k_norm"][0], np.float32), 2))
    invf = (np.float32(10000.0) ** (-np.arange(32, dtype=np.float32) * np.float32(2.0 / 64))).astype(np.float32)
    put("invf", np.tile(invf, 4))

    rows = np.zeros((128, NROWS), np.float32)
    rows[:, 0:1024] = b_in[2048:3072][None, :]
    rows[:, 1024:1032] = b_in[3072:3080][None, :]
    rows[:, 1032:1544] = np.asarray(inp["ml_norm"][0], np.float32)[None, :]
    rows[:, 1544:1560] = np.asarray(inp["at_sinks"][0], np.float32)[None, :]

    cons = np.zeros((128, NCONS), np.float32)
    i = np.arange(128)
    cons[:, 0:128] = np.eye(128, dtype=np.float32)
    cons[:, 128:256] = 1.0
    cons[:, 256:384] = (i[:, None] <= i[None, :]).astype(np.float32)
    cons[:, 384:512] = (i[:, None] > i[None, :]).astype(np.float32)
    rt = np.zeros((128, 128), np.float32)
    for blk in range(2):
        for j in range(32):
            rt[blk * 64 + j + 32, blk * 64 + j] = -1.0
            rt[blk * 64 + j, blk * 64 + j + 32] = 1.0
    cons[:, 512:640] = rt
    cons[:, 640:768] = (i[:, None] // 64 == i[None, :] // 64).astype(np.float32)

    def bdiag(w):
        w = np.asarray(w, np.float32)
        out = np.zeros((128, 4, 128), np.float32)
        for c in range(4):
            for j in range(2):
                out[j * 64:(j + 1) * 64, c, j * 64:(j + 1) * 64] = w[2 * c + j]
        return out

    wqkv = np.asarray(inp["at_w_qkv"][0], np.float32)
    wk = wqkv[:, 1024:1280]
    wkd = np.concatenate([np.concatenate([wk[:, g * 64:(g + 1) * 64]] * 2, axis=1) for g in range(4)], axis=1)
    shared = {
        "cols": cols, "rows": rows, "cons": cons,
        "bda": bdiag(inp["rg_w_a"][0]), "bdx": bdiag(inp["rg_w_x"][0]),
        "wif": np.ascontiguousarray(np.asarray(inp["hy_w_in"][0], np.float32)[:, 3072:3080]),
        "w_in": np.ascontiguousarray(np.asarray(inp["hy_w_in"][0], np.float32)),
        "hy_w_out": np.ascontiguousarray(np.asarray(inp["hy_w_out"][0], np.float32)),
        "wq": np.ascontiguousarray(wqkv[:, 0:1024]),
        "wkd": np.ascontiguousarray(wkd),
        "wv": np.ascontiguousarray(wqkv[:, 1280:1536]),
        "at_w_out": np.ascontiguousarray(np.asarray(inp["at_w_out"][0], np.float32)),
        "ff_w_up": np.ascontiguousarray(np.asarray(inp["ff_w_up"], np.float32)),
        "ff_w_down": np.ascontiguousarray(np.asarray(inp["ff_w_down"], np.float32)),
        "ple_w_gate": np.ascontiguousarray(np.asarray(inp["ple_w_gate"], np.float32)),
        "ple_w_proj": np.ascontiguousarray(np.asarray(inp["ple_w_proj"], np.float32)),
    }
    return shared


class Prog:
    def __init__(self, nblk=8, nstage=6, skip=()):
        self.skip = set(skip)
        self.debug = False
        self.reorder = True
        self.keep = ()
        self.dbgkeys = []
        self.nblk = nblk
        self.nstage = nstage
        nc = self.nc = bass.Bass("TRN2", target_bir_lowering=False)
        self.s = Sched(nc)
        dt = nc.dram_tensor
        self.d = {}
        for name, shape, ty in [
            ("xT", [D, S], F32), ("pT", [2, 256, S], F32), ("pos", [1, S], I32),
            ("cols", [128, NCOLS], F32), ("rows", [128, NROWS], F32), ("cons", [128, NCONS], F32),
            ("bda", [128, 4, 128], F32), ("bdx", [128, 4, 128], F32), ("wif", [D, 8], F32),
            ("w_in", [D, 3080], F32), ("hy_w_out", [D, D], F32),
            ("wq", [D, 1024], F32), ("wkd", [D, 512], F32), ("wv", [D, 256], F32), ("at_w_out", [D, D], F32),
            ("ff_w_up", [2, D, 6144], F32), ("ff_w_down", [2, 3072, D], F32),
            ("ple_w_gate", [2, D, D], F32), ("ple_w_proj", [2, 256, D], F32),
        ]:
            self.d[name] = dt(name, shape, ty, kind="ExternalInput").ap()
        self.d["outT"] = dt("outT", [D, S], F32, kind="ExternalOutput").ap()
        self._n = 0
        self.rot = {}
        self.bankc = 0
        self.alloc()
        self.loads = []
        self.nload = 0
        self.nissued = 0

    def sb(self, shape, ty=F32, name=None):
        self._n += 1
        return self.nc.alloc_sbuf_tensor("%s_%d" % (name or "t", self._n), list(shape), ty).ap()

    def alloc(self):
        sb = self.sb
        self.ps = [self.nc.alloc_psum_tensor("ps%d" % i, [128, 512], F32).ap() for i in range(8)]
        self.cols = sb([128, NCOLS], F32, "cols")
        self.rows = sb([128, NROWS], F32, "rows")
        self.cons = sb([128, NCONS], F32, "cons")
        self.consb = sb([128, NCONS], BF16, "consb")
        self.bda = sb([128, 4, 128], BF16, "bda")
        self.bdx = sb([128, 4, 128], BF16, "bdx")
        self.wif = sb([128, 8, 8], BF16, "wif")
        self.misc = sb([128, 16], F32, "misc")
        self.cb2 = sb([128, 12], F32, "cb2")
        self.cA = sb([128, 8], F32, "cA")
        self.esink = sb([128, 16], F32, "esink")
        self.mask4 = sb([128, 512], BF16, "mask4")
        self.x = [sb([128, 8, TB], F32, "x%d" % i) for i in range(2)]
        self.h = sb([128, 8, TB], BF16, "h")
        self.rstd = sb([128, TB], F32, "rstd")
        self.slots = [sb([128, 4096], BF16, "ws%d" % i) for i in range(NSLOT)]
        self.yfm = sb([128, 8, TB], BF16, "yfm")
        self.tf = [sb([128, 520], F32, "tf%d" % i) for i in range(6)]
        self.cvin = [sb([128, 520], F32, "cvin%d" % i) for i in range(2)]
        self.cvacc = [sb([128, 520], F32, "cvacc%d" % i) for i in range(2)]
        self.tb = [sb([128, 520], BF16, "tb%d" % i) for i in range(4)]
        self.rg_halo = sb([128, 4, 3], F32, "rghalo")
        self.rg_h = sb([128, 4], F32, "rgh")
        self.ml_halo = sb([128, 8, 3], F32, "mlhalo")
        self.qk8 = sb([128, 8, TB], BF16, "qk8")
        self.qfm = self.qk8[:, 0:4, :]
        self.kfm = self.qk8[:, 4:8, :]
        self.qrope = self.qk8
        self.vaug = sb([128, NT, 4, 129], BF16, "vaug")
        self.og = sb([128, NT, 512], BF16, "og")
        self.ifs = sb([128, NT, 8], F32, "ifs")
        self.Cf = sb([128, 4, 129], F32, "Cf")
        self.Cb = sb([128, 4, 129], BF16, "Cb")
        self.sm = [sb([128, 64], F32, "sm%d" % i) for i in range(4)]
        self.Eb = [sb([128, 4, 128], F32, "Eb%d" % i) for i in range(1)]
        self.rhsall = [sb([128, 4, 128], F32, "rhsall%d" % i) for i in range(1)]
        self.hm = [sb([128, 4, 128], F32, "hm%d" % i) for i in range(2)]
        self.ytok = [sb([128, 1024], BF16, "ytok%d" % i) for i in range(1)]
        self.ff_halo = [sb([128, 24, 2], F32, "ffhalo%d" % l) for l in range(2)]
        self.hid = sb([128, 24, TB], BF16, "hid")
        self.xsq = self.hid[:, 0:8, :]
        self.pblk = [sb([128, 2, TB], BF16, "pblk%d" % i) for i in range(2)]
        self.posi = sb([128, TB], I32, "posi")
        self.cosb = sb([128, TB], F32, "cos")
        self.sinb = sb([128, TB], F32, "sin")
        self.kbuf = sb([128, 8, 128 + TB], BF16, "kbuf")
        self.vbuf = sb([128, NT + 1, 4, 65], BF16, "vbuf")

    def rotbuf(self, name, lst):
        i = self.rot.get(name, 0)
        self.rot[name] = i + 1
        j = i % len(lst)
        return lst[j], (name, j)

    def bank(self):
        i = self.bankc % 8
        self.bankc += 1
        return self.ps[i], ("ps", i)

    def col(self, name, j=0):
        o, w = COLS[name]
        return self.cols[:, o + j:o + j + 1]

    def con(self, name, bf=True):
        o = CONS[name]
        return (self.consb if bf else self.cons)[:, o:o + 128]

    def mm(self, out, lhsT, rhs, start, stop, r, w):
        n = _nfree(rhs)
        dur = max(0.06, n / 1940.0)
        if rhs.dtype == F32:
            dur *= 4
        self.s.op("pe", lambda e: e.matmul(out, lhsT=lhsT, rhs=rhs, start=start, stop=stop), r, w, dur=dur)
        self.s.ops[-1]["grp"] = (bool(start), bool(stop))

    def tr(self, out, in_, ident, r, w):
        self.s.op("pe", lambda e: e.transpose(out, in_, ident), r, w, dur=0.064 + 128 / 1940.0)

    def act(self, out, in_, func, r, w, bias=None, scale=None, accum=None):
        kw = {}
        if bias is not None:
            kw["bias"] = bias
        if scale is not None:
            kw["scale"] = scale
        if accum is not None:
            kw["accum_out"] = accum
        self.s.op("act", lambda e: e.activation(out=out, in_=in_, func=func, **kw), r, w, dur=0.22 + _nfree(out) * 0.0006)
        self.s.ops[-1]["aset"] = ACT_SET.get(func)

    def tt(self, out, a, b, op, r, w, eng="dve"):
        dur = (0.15 + _nfree(out) * 0.0022) if eng == "pool" else (0.1 + _nfree(out) * 0.00105)
        self.s.op(eng, lambda e: e.tensor_tensor(out=out, in0=a, in1=b, op=op), r, w, dur=dur)

    def ts(self, out, a, s1, s2, op0, op1, r, w, eng="dve"):
        if op1 is None:
            self.s.op(eng, lambda e: e.tensor_scalar(out=out, in0=a, scalar1=s1, scalar2=None, op0=op0), r, w,
                      dur=0.1 + _nfree(out) * 0.00105)
        else:
            self.s.op(eng, lambda e: e.tensor_scalar(out=out, in0=a, scalar1=s1, scalar2=s2, op0=op0, op1=op1), r, w,
                      dur=0.1 + _nfree(out) * 0.00105)

    def stt(self, out, a, sc, b, op0, op1, r, w):
        self.s.op("dve", lambda e: e.scalar_tensor_tensor(out=out, in0=a, scalar=sc, in1=b, op0=op0, op1=op1), r, w,
                  dur=0.1 + _nfree(out) * 0.00105)

    def cp(self, out, in_, r, w, eng="dve"):
        if eng == "act":
            self.s.op("act", lambda e: e.activation(out=out, in_=in_, func=AF.Identity), r, w, dur=0.22 + _nfree(out) * 0.0006)
        else:
            self.s.op(eng, lambda e: e.tensor_copy(out=out, in_=in_), r, w, dur=0.1 + _nfree(out) * 0.00105)

    def rcp(self, out, in_, r, w):
        self.s.op("dve", lambda e: e.reciprocal(out=out, in_=in_), r, w, dur=0.1 + _nfree(out) * 0.0024)

    def mset(self, out, val, w, eng="dve"):
        self.s.op(eng, lambda e: e.memset(out, val), (), w, dur=0.1 + _nfree(out) * 0.0005)

    def dma(self, q, out, in_, sem, r, w):
        nb = _nfree(out) * int(out.shape[0]) * (4 if in_.dtype == F32 else 2)
        self.s.dma(q, lambda e: e.dma_start(out=out, in_=in_), sem, r, w, nbytes=nb)

    def dump(self, name, ap, rkeys):
        if not self.debug:
            return
        t = self.nc.dram_tensor("dbg_" + name, list(ap.shape), ap.dtype, kind="ExternalOutput").ap()
        self.dma("sp", t, ap, "dbgsem_" + name, rkeys, [("dbg", name)])
        self.dbgkeys.append(("dbg", name))

    def plan_loads(self):
        L = []
        d = self.d

        def kview(ap2d, kc):
            return ap2d.rearrange("(k p) n -> p k n", p=128)

        for b in range(self.nblk):
            ns = self.nstage
            if ns >= 1 and "mix0" not in self.skip:
                for g in (2, 3, 4, 5, 0, 1):
                    L.append(("w_in%d" % g, kview(d["w_in"], 8)[:, :, g * 512:(g + 1) * 512], 8, 512))
                for g in range(2):
                    L.append(("hyout%d" % g, kview(d["hy_w_out"], 8)[:, :, g * 512:(g + 1) * 512], 8, 512))
            for l in range(2):
                if l == 1 and ns >= 4 and "mix1" not in self.skip:
                    for g in range(2):
                        L.append(("wq%d" % g, kview(d["wq"], 8)[:, :, g * 512:(g + 1) * 512], 8, 512))
                    L.append(("wkd", kview(d["wkd"], 8), 8, 512))
                    L.append(("wv", kview(d["wv"], 8), 8, 256))
                    for g in range(2):
                        L.append(("atout%d" % g, kview(d["at_w_out"], 8)[:, :, g * 512:(g + 1) * 512], 8, 512))
                if ns >= 2 + 3 * l and ("ffn%d" % l) not in self.skip:
                    up = kview(d["ff_w_up"][l], 8)
                    for g in range(6):
                        L.append(("upg%d_%d" % (l, g), up[:, :, g * 512:(g + 1) * 512], 8, 512))
                        L.append(("upu%d_%d" % (l, g), up[:, :, 3072 + g * 512:3072 + (g + 1) * 512], 8, 512))
                    dn = kview(d["ff_w_down"][l], 24)
                    for m in range(8):
                        L.append(("dn%d_%d" % (l, m), dn[:, :, m * 128:(m + 1) * 128], 24, 128))
                if ns >= 3 + 3 * l and ("ple%d" % l) not in self.skip:
                    pg = kview(d["ple_w_gate"][l], 8)
                    pp = kview(d["ple_w_proj"][l], 2)
                    for g in range(2):
                        L.append(("pp%d_%d" % (l, g), pp[:, :, g * 512:(g + 1) * 512], 2, 512))
                        L.append(("pg%d_%d" % (l, g), pg[:, :, g * 512:(g + 1) * 512], 8, 512))
        self.loads = L

    def issue_load(self):
        if self.nissued >= len(self.loads):
            return
        i = self.nissued
        self.nissued += 1
        name, src, kc, n = self.loads[i]
        sl = i % NSLOT
        dst = self.slots[sl][:, 0:kc * n].rearrange("p (k n) -> p k n", k=kc)
        self.dma("pool", dst, src, "wsem%d" % sl, (), [("ws", sl)])

    def getw(self, name):
        i = self.nload
        self.nload += 1
        lname, src, kc, n = self.loads[i]
        assert lname == name, (lname, name)
        sl = i % NSLOT
        view = self.slots[sl][:, 0:kc * n].rearrange("p (k n) -> p k n", k=kc)
        return view, ("ws", sl)

    def donew(self):
        self.issue_load()

    def setup(self):
        d = self.d
        K = []
        for nm, dst in [("cols", self.cols), ("rows", self.rows), ("cons", self.cons)]:
            self.dma("sp", dst, d[nm], "setup", (), [nm])
            K.append(nm)
        self.dma("pool", self.bda, d["bda"], "setup2", (), ["bda"])
        self.dma("pool", self.bdx, d["bdx"], "setup2", (), ["bdx"])
        self.dma("pool", self.wif, d["wif"].rearrange("(k p) n -> p k n", p=128), "setup2", (), ["wif"])
        self.mset(self.misc[:, 0:1], EPS, ["misc"])
        self.mset(self.misc[:, 1:2], 1.0, ["misc"])
        self.mset(self.misc[:, 2:3], 0.0, ["misc"])
        self.mset(self.misc[:, 3:4], float(np.pi / 2), ["misc"])
        self.cp(self.consb, self.cons, ["cons"], ["consb"])
        for j in range(4):
            src = self.con("mprev") if j % 2 == 0 else self.con("tri")
            self.ts(self.mask4[:, j * 128:(j + 1) * 128], src, 30000.0, -30000.0, ALU.mult, ALU.add, ["consb"], ["mask4"])
        o, w = COLS["rglam"]
        t = self.sm[0]
        self.act(t[:, 0:4], self.cols[:, o:o + 4], AF.Exp, ["cols"], ["sm0"], scale=-1.0)
        self.act(t[:, 4:8], t[:, 0:4], AF.Ln, ["sm0", "misc"], ["sm0"], bias=self.misc[:, 1:2])
        self.ts(self.cA[:, 0:4], t[:, 4:8], -8.0, None, ALU.mult, None, ["sm0"], ["cA"])
        self.ts(self.cA[:, 4:8], t[:, 4:8], -16.0, None, ALU.mult, None, ["sm0"], ["cA"])
        bino, _ = COLS["bin"]
        for i in range(12):
            if i < 4:
                wl = self.cols[:, COLS["rgcw"][0] + i * 4 + 3:COLS["rgcw"][0] + i * 4 + 4]
                cb = self.col("rgcb", i)
                bi = self.cols[:, bino + i:bino + i + 1]
            else:
                ci = i - 4
                wl = self.cols[:, COLS["mlcw"][0] + ci * 4 + 3:COLS["mlcw"][0] + ci * 4 + 4]
                cb = self.col("mlcb", ci)
                bi = self.cols[:, bino + 8 + ci:bino + 9 + ci]
            self.stt(self.cb2[:, i:i + 1], bi, wl, cb, ALU.mult, ALU.add, ["cols"], ["cb2"])
        o, w = ROWS["snk"]
        self.act(self.esink, self.rows[:, o:o + 16], AF.Exp, ["rows"], ["esink"])
        for buf, key in [(self.rg_halo, "rghalo"), (self.rg_h, "rgh"), (self.ml_halo, "mlhalo"),
                         (self.ff_halo[0], "ffhalo0"),
                         (self.ff_halo[1], "ffhalo1"), (self.vbuf, "vbuf"),
]:
            self.mset(buf, 0.0, [key])
        for g in range(4):
            self.mset(self.kbuf[:, 2 * g:2 * g + 2, :], 0.0, [("kbuf", g)])
        for hd in range(4):
            self.mset(self.Cf[:, hd, :], 0.0, [("Cf", hd)])
            self.mset(self.Cb[:, hd, :], 0.0, [("Cb", hd)])
        for q_ in range(NT):
            self.mset(self.vaug[:, q_, :, 128:129], 1.0, [("vaug", q_)])
        self.mset(self.vbuf[:, :, :, 64:65], 1.0, ["vbuf"])

    def rmsnorm(self, xb, xk, gname):
        for kc in range(8):
            self.act(self.xsq[:, kc, :], xb[:, kc, :], AF.Square, [(xk, kc)], ["hid"])
        ps, pk = self.bank()
        for kc in range(8):
            self.mm(ps, self.con("ones"), self.xsq[:, kc, :], kc == 0, kc == 7, ["hid", "consb"], [pk])
        self.act(self.rstd, ps, AF.Ln, [pk, "misc"], ["rstd"], bias=self.misc[:, 0:1], scale=1.0 / D)
        self.act(self.rstd, self.rstd, AF.Exp, ["rstd"], ["rstd"], scale=-0.5)
        for kc in range(8):
            self.stt(self.h[:, kc, :], xb[:, kc, :], self.col(gname, kc), self.rstd, ALU.mult, ALU.mult,
                     [(xk, kc), "rstd", "cols"], ["h"])

    def conv(self, ps, pk, halo, hk, wname, widx, ktaps, bias_ap, pre_bias=None, bias2=None):
        hl = ktaps - 1
        buf, bk = self.rotbuf("cvin", self.cvin)
        acc, ak = self.rotbuf("cvacc", self.cvacc)
        self.cp(buf[:, 0:hl], halo, [hk], [bk])
        if pre_bias is not None:
            self.act(buf[:, hl:hl + TB], ps, AF.Identity, [pk, "cols"], [bk], bias=pre_bias)
        else:
            self.cp(buf[:, hl:hl + TB], ps, [pk], [bk], eng="act")
        self.cp(halo, buf[:, TB:TB + hl], [bk], [hk])
        wo, _ = COLS[wname]
        wc = lambda j: self.cols[:, wo + widx * ktaps + j:wo + widx * ktaps + j + 1]
        self.act(acc[:, 0:TB], ps, AF.Identity, [pk, "cols", "cb2"], [ak], bias=(bias2 if bias2 is not None else bias_ap),
                 scale=wc(ktaps - 1))
        for j in range(ktaps - 2, -1, -1):
            self.stt(acc[:, 0:TB], buf[:, j:j + TB], wc(j), acc[:, 0:TB], ALU.mult, ALU.add, [bk, ak, "cols"], [ak])
        return acc, ak

    def proj(self, wv, wk, cols0, actb, ak, nk):
        ps, pk = self.bank()
        for kc in range(nk):
            self.mm(ps, wv[:, kc, cols0:cols0 + 128], actb[:, kc, :], kc == 0, kc == nk - 1,
                    [wk] + (list(ak) if isinstance(ak, list) else [ak]), [pk])
        return ps, pk

    def mixer0(self, blk, xb, xk):
        self.rmsnorm(xb, xk, "nmix0")
        bino, _ = COLS["bin"]
        for gi, dst, dk in [(2, self.qfm, 0), (3, self.kfm, 4)]:
            wv, wk = self.getw("w_in%d" % gi)
            for c in range(4):
                ci = (gi - 2) * 4 + c
                ps, pk = self.proj(wv, wk, c * 128, self.h, "h", 8)
                acc, ak = self.conv(ps, pk, self.ml_halo[:, ci, :], "mlhalo", "mlcw", ci, 4,
                                    self.col("mlcb", ci), pre_bias=self.cols[:, bino + 8 + ci:bino + 9 + ci],
                                    bias2=self.cb2[:, 4 + ci:5 + ci])
                self.act(dst[:, c, :], acc[:, 0:TB], AF.Silu, [ak], [("qk8", dk + c)])
            self.donew()
        io, _ = ROWS["bif"]
        for tt_ in range(NT):
            ps, pk = self.bank()
            for kc in range(8):
                self.mm(ps[:, 0:8], self.h[:, kc, tt_ * 128:(tt_ + 1) * 128], self.wif[:, kc, :], kc == 0, kc == 7,
                        ["h", "wif"], [pk])
            self.tt(self.ifs[:, tt_, :], ps[:, 0:8], self.rows[:, io:io + 8], ALU.add, [pk, "rows"], [("ifs", tt_)])
        wv4, wk4 = self.getw("w_in4")
        wv5, wk5 = self.getw("w_in5")
        bo, _ = ROWS["bvo"]
        mo, _ = ROWS["mln"]
        for tt_ in range(NT):
            ps, pk = self.bank()
            for kc in range(8):
                self.mm(ps, self.h[:, kc, tt_ * 128:(tt_ + 1) * 128], wv4[:, kc, :], kc == 0, kc == 7, ["h", wk4], [pk])
            self.tt(self.vaug[:, tt_, :, 0:128], ps.rearrange("p (h e) -> p h e", h=4),
                    self.rows[:, bo:bo + 512].rearrange("p (h e) -> p h e", h=4), ALU.add,
                    [pk, "rows"], [("vaug", tt_)])
            ps, pk = self.bank()
            for kc in range(8):
                self.mm(ps, self.h[:, kc, tt_ * 128:(tt_ + 1) * 128], wv5[:, kc, :], kc == 0, kc == 7, ["h", wk5], [pk])
            t1, t1k = self.rotbuf("tf", self.tf)
            self.tt(t1[:, 0:512], ps, self.rows[:, bo + 512:bo + 1024], ALU.add, [pk, "rows"], [t1k])
            self.act(t1[:, 0:512], t1[:, 0:512], AF.Sigmoid, [t1k], [t1k])
            self.tt(self.og[:, tt_, :], t1[:, 0:512], self.rows[:, mo:mo + 512], ALU.mult, [t1k, "rows"], [("og", tt_)])
        self.donew()
        self.donew()
        cq = float(128.0 ** -0.5)
        for tt_ in range(NT):
            tsl = slice(tt_ * 128, (tt_ + 1) * 128)
            sm, smk = self.rotbuf("sm", self.sm)
            self.act(sm[:, 0:4], self.ifs[:, tt_, 4:8], AF.Exp, [("ifs", tt_)], [smk], scale=-1.0)
            self.act(sm[:, 4:8], sm[:, 0:4], AF.Ln, [smk, "misc"], [smk], bias=self.misc[:, 1:2])
            self.ts(sm[:, 8:12], sm[:, 4:8], -1.0, None, ALU.mult, None, [smk], [smk])
            ra, rak = self.rotbuf("rhsall", self.rhsall)
            for hd in range(4):
                self.ts(ra[:, hd, :], self.con("tri", bf=False), sm[:, 8 + hd:9 + hd], None, ALU.mult, None,
                        [smk, "cons"], [rak])
            psb, pbk = self.bank()
            self.mm(psb[:, 0:4], self.con("tri", bf=False), sm[:, 8:12], True, True, ["cons", smk], [pbk])
            pse, pek = self.bank()
            self.mm(pse, self.con("ones", bf=False), ra.rearrange("p h t -> p (h t)"), True, True, ["cons", rak], [pek])
            Eb, ebk = self.rotbuf("Eb", self.Eb)
            self.act(Eb.rearrange("p h t -> p (h t)"), pse, AF.Exp, [pek], [ebk])
            self.tt(sm[:, 12:16], self.ifs[:, tt_, 0:4], psb[:, 0:4], ALU.subtract, [("ifs", tt_), pbk], [smk])
            self.act(sm[:, 16:20], sm[:, 12:16], AF.Exp, [smk], [smk])
            self.tt(sm[:, 20:24], sm[:, 16:20], Eb[:, :, 127], ALU.mult, [smk, ebk], [smk])
            hm, hmk = self.rotbuf("hm", self.hm)
            for hd in range(4):
                qt, qtk = self.rotbuf("tb", self.tb)
                self.stt(qt[:, 0:128], self.qfm[:, hd, tsl], cq, Eb[:, hd, :], ALU.mult, ALU.mult, [("qk8", hd), ebk], [qtk])
                pss, psk = self.bank()
                self.mm(pss[:, 0:128], self.kfm[:, hd, tsl], qt[:, 0:128], True, True, [("qk8", 4 + hd), qtk], [psk])
                sq, sqk = self.rotbuf("tb", self.tb)
                self.stt(sq[:, 0:128], pss[:, 0:128], sm[:, 16 + hd:17 + hd], self.con("tri", bf=False),
                         ALU.mult, ALU.mult, [psk, smk, "cons"], [sqk])
                psn, pnk = self.bank()
                self.mm(psn[:, 0:129], sq[:, 0:128], self.vaug[:, tt_, hd, :], True, False, [sqk, ("vaug", tt_)], [pnk])
                self.mm(psn[:, 0:129], qt[:, 0:128], self.Cb[:, hd, :], False, True, [qtk, ("Cb", hd)], [pnk])
                self.ts(sm[:, 40 + hd:41 + hd], psn[:, 128:129], 1.0, None, ALU.max, None, [pnk], [(smk, hd)])
                self.stt(sm[:, 24 + hd:25 + hd], psn[:, 128:129], -1.0, sm[:, 40 + hd:41 + hd], ALU.mult, ALU.max, [pnk, (smk, hd)], [(smk, hd)])
                self.rcp(sm[:, 28 + hd:29 + hd], sm[:, 24 + hd:25 + hd], [(smk, hd)], [(smk, hd)])
                self.ts(hm[:, hd, :], psn[:, 0:128], sm[:, 28 + hd:29 + hd], None, ALU.mult, None, [pnk, (smk, hd)], [(hmk, hd)])
                pst, ptk = self.bank()
                pstb = pst.bitcast(BF16)
                self.tr(pstb[:, 0:128], self.kfm[:, hd, tsl], self.con("ident"), [("qk8", 4 + hd), "consb"], [ptk])
                kw, kwk = self.rotbuf("tb", self.tb)
                self.act(kw[:, 0:128], pstb[:, 0:128], AF.Identity, [ptk, smk], [kwk], scale=sm[:, 20 + hd:21 + hd])
                psc, pck = self.bank()
                self.mm(psc[:, 0:129], kw[:, 0:128], self.vaug[:, tt_, hd, :], True, True, [kwk, ("vaug", tt_)], [pck])
                self.stt(self.Cf[:, hd, :], self.Cf[:, hd, :], Eb[:, hd, 127:128], psc[:, 0:129], ALU.mult, ALU.add,
                         [("Cf", hd), ebk, pck], [("Cf", hd)])
                self.cp(self.Cb[:, hd, :], self.Cf[:, hd, :], [("Cf", hd)], [("Cb", hd)], eng="act")
            if blk == 0 and tt_ == 0:
                self.dump("sm", sm, [smk] + [(smk, q_) for q_ in range(4)])
                self.dump("Eb", Eb, [ebk])
                self.dump("ra", ra, [rak])
                self.dump("hm", hm, [(hmk, q_) for q_ in range(4)])
            jb, jk = self.rotbuf("tf", self.tf)
            junk = jb[:, 0:512].rearrange("p (h e) -> p h e", h=4)
            self.act(junk, hm, AF.Square, [(hmk, q_) for q_ in range(4)], [jk])
            self.s.op("dve", (lambda o_, i_: (lambda e: e.tensor_reduce(out=o_, in_=i_, axis=mybir.AxisListType.X, op=ALU.add)))(
                sm[:, 32:36], junk), [jk], [smk], dur=0.65)
            self.act(sm[:, 36:40], sm[:, 32:36], AF.Ln, [smk, "misc"], [smk], bias=self.misc[:, 0:1], scale=1.0 / 128)
            self.act(sm[:, 36:40], sm[:, 36:40], AF.Exp, [smk], [smk], scale=-0.5)
            yt, ytk = self.rotbuf("ytok", self.ytok)
            for hd in range(4):
                self.stt(yt[:, hd * 128:(hd + 1) * 128], hm[:, hd, :], sm[:, 36 + hd:37 + hd],
                         self.og[:, tt_, hd * 128:(hd + 1) * 128], ALU.mult, ALU.mult, [(hmk, hd), smk, ("og", tt_)], [ytk])
            psy, pyk = self.bank()
            psyb = psy.bitcast(BF16)
            for hd in range(4):
                self.tr(psyb[:, hd * 128:(hd + 1) * 128], yt[:, hd * 128:(hd + 1) * 128], self.con("ident"),
                        [ytk, "consb"], [pyk])
            self.cp(self.yfm[:, 4:8, tsl], psyb[:, 0:512].rearrange("p (h t) -> p h t", h=4), [pyk], [("yfm", 4 + q_) for q_ in range(4)], eng="act")
        w0, k0 = self.getw("w_in0")
        w1, k1 = self.getw("w_in1")
        for c in range(4):
            ps, pk = self.proj(w0, k0, c * 128, self.h, "h", 8)
            xc, xck = self.conv(ps, pk, self.rg_halo[:, c, :], "rghalo", "rgcw", c, 4,
                                self.col("rgcb", c), pre_bias=self.cols[:, bino + c:bino + c + 1], bias2=self.cb2[:, c:c + 1])
            xcb, xcbk = self.rotbuf("tb", self.tb)
            self.cp(xcb[:, 0:TB], xc[:, 0:TB], [xck], [xcbk], eng="act")
            psr, prk = self.bank()
            self.mm(psr, self.bda[:, c, :], xcb[:, 0:TB], True, True, ["bda", xcbk], [prk])
            psi, pik = self.bank()
            self.mm(psi, self.bdx[:, c, :], xcb[:, 0:TB], True, True, ["bdx", xcbk], [pik])
            rr, rk = self.rotbuf("tf", self.tf)
            ii, ik = self.rotbuf("tf", self.tf)
            self.act(rr[:, 0:TB], psr, AF.Sigmoid, [prk, "cols"], [rk], bias=self.col("rgba", c))
            self.act(ii[:, 0:TB], psi, AF.Sigmoid, [pik, "cols"], [ik], bias=self.col("rgbx", c))
            aa, akk = self.rotbuf("tf", self.tf)
            a2, a2k = self.rotbuf("tf", self.tf)
            self.act(aa[:, 0:TB], rr[:, 0:TB], AF.Exp, [rk, "cA"], [akk], scale=self.cA[:, c:c + 1])
            self.act(a2[:, 0:TB], rr[:, 0:TB], AF.Exp, [rk, "cA"], [a2k], scale=self.cA[:, 4 + c:5 + c])
            self.act(a2[:, 0:TB], a2[:, 0:TB], AF.Ln, [a2k, "misc"], [a2k], bias=self.misc[:, 1:2], scale=-1.0)
            self.act(a2[:, 0:TB], a2[:, 0:TB], AF.Exp, [a2k], [a2k], scale=0.5)
            self.tt(ii[:, 0:TB], ii[:, 0:TB], xc[:, 0:TB], ALU.mult, [ik, xck], [ik])
            self.tt(ii[:, 0:TB], ii[:, 0:TB], a2[:, 0:TB], ALU.mult, [ik, a2k], [ik])
            hs, hk = self.rotbuf("tf", self.tf)
            self.s.op("dve", (lambda o_, a_, u_, i_: (lambda e: e.tensor_tensor_scan(
                out=o_, data0=a_, data1=u_, initial=i_, op0=ALU.mult, op1=ALU.add)))(
                hs[:, 0:TB], aa[:, 0:TB], ii[:, 0:TB], self.rg_h[:, c:c + 1]), [akk, ik, "rgh"], [hk], dur=1.2)
            self.cp(self.rg_h[:, c:c + 1], hs[:, TB - 1:TB], [hk], ["rgh"])
            psg, pgk = self.proj(w1, k1, c * 128, self.h, "h", 8)
            gg, ggk = self.rotbuf("tf", self.tf)
            self.act(gg[:, 0:TB], psg, AF.Gelu_apprx_tanh, [pgk, "cols"], [ggk],
                     bias=self.cols[:, bino + 4 + c:bino + 5 + c])
            self.tt(self.yfm[:, c, :], hs[:, 0:TB], gg[:, 0:TB], ALU.mult, [hk, ggk], [("yfm", c)])
        self.donew()
        self.donew()
        if blk == 0:
            self.dump("yfm", self.yfm, [("yfm", q_) for q_ in range(8)])
            self.dump("qk8", self.qk8, [("qk8", q_) for q_ in range(8)])
            self.dump("vaug", self.vaug, [("vaug", q_) for q_ in range(NT)])
            self.dump("og", self.og, [("og", q_) for q_ in range(NT)])
            self.dump("ifs", self.ifs, [("ifs", q_) for q_ in range(NT)])
            self.dump("Cf", self.Cf, [("Cf", q_) for q_ in range(4)])
        for g in range(2):
            wv, wk = self.getw("hyout%d" % g)
            for m in range(4):
                ps, pk = self.proj(wv, wk, m * 128, self.yfm, [("yfm", q_) for q_ in range(8)], 8)
                mm_ = g * 4 + m
                self.tt(xb[:, mm_, :], xb[:, mm_, :], ps, ALU.add, [(xk, mm_), pk], [(xk, mm_)])
            self.donew()

    def ffn(self, l, xb, xk):
        self.rmsnorm(xb, xk, "nffn%d" % l)
        for g in range(6):
            wg, wgk = self.getw("upg%d_%d" % (l, g))
            wu, wuk = self.getw("upu%d_%d" % (l, g))
            for m in range(4):
                j = g * 4 + m
                psg, pgk = self.proj(wg, wgk, m * 128, self.h, "h", 8)
                psu, puk = self.proj(wu, wuk, m * 128, self.h, "h", 8)
                acc, ak = self.conv(psg, pgk, self.ff_halo[l][:, j, :], "ffhalo%d" % l, "ffcw%d" % l, j, 3,
                                    self.col("ffcb%d" % l, j))
                ge, gk = self.rotbuf("tf", self.tf)
                self.act(ge[:, 0:TB], acc[:, 0:TB], AF.Gelu_apprx_tanh, [ak], [gk])
                self.tt(self.hid[:, j, :], ge[:, 0:TB], psu, ALU.mult, [gk, puk], ["hid"])
            self.donew()
            self.donew()
        for m in range(8):
            wv, wk = self.getw("dn%d_%d" % (l, m))
            ps, pk = self.bank()
            for j in range(24):
                self.mm(ps, wv[:, j, :], self.hid[:, j, :], j == 0, j == 23, [wk, "hid"], [pk])
            self.tt(xb[:, m, :], xb[:, m, :], ps, ALU.add, [(xk, m), pk], [(xk, m)])
            self.donew()

    def ple(self, l, blk, xb, xk):
        self.rmsnorm(xb, xk, "nple%d" % l)
        for g in range(2):
            wp, wpk = self.getw("pp%d_%d" % (l, g))
            wv, wk = self.getw("pg%d_%d" % (l, g))
            for m4 in range(4):
                m = g * 4 + m4
                ps, pk = self.proj(wv, wk, m4 * 128, self.h, "h", 8)
                gt, gk = self.rotbuf("tf", self.tf)
                self.act(gt[:, 0:TB], ps, AF.Sigmoid, [pk], [gk])
                ps2, pk2 = self.bank()
                for kc in range(2):
                    self.mm(ps2, wp[:, kc, m4 * 128:(m4 + 1) * 128], self.pblk[l][:, kc, :], kc == 0, kc == 1,
                            [wpk, ("pblk", l)], [pk2])
                self.tt(gt[:, 0:TB], gt[:, 0:TB], ps2, ALU.mult, [gk, pk2], [gk])
                self.tt(xb[:, m, :], xb[:, m, :], gt[:, 0:TB], ALU.add, [(xk, m), gk], [(xk, m)])
            self.donew()
            self.donew()

    def rope_tables(self, blk):
        t0 = blk * TB
        self.dma("sp", self.posi, self.d["pos"][0, t0:t0 + TB].partition_broadcast(128), "possem", (), ["posi"])
        TWO_PI = float(2 * np.pi)
        for which, dst, dk in [(0, self.sinb, "sin"), (1, self.cosb, "cos")]:
            ang, ak = self.rotbuf("tf", self.tf)
            nn, nk = self.rotbuf("tf", self.tf)
            ni = nn.bitcast(I32)
            self.cp(ang[:, 0:TB], self.posi, ["posi"], [ak])
            if which == 0:
                self.ts(ang[:, 0:TB], ang[:, 0:TB], self.col("invf"), None, ALU.mult, None, [ak, "cols"], [ak])
            else:
                self.ts(ang[:, 0:TB], ang[:, 0:TB], self.col("invf"), float(np.pi / 2), ALU.mult, ALU.add, [ak, "cols"], [ak])
            self.ts(ni[:, 0:TB], ang[:, 0:TB], 1.0 / TWO_PI, None, ALU.mult, None, [ak], [nk])
            red, rk = self.rotbuf("tf", self.tf)
            self.cp(red[:, 0:TB], ni[:, 0:TB], [nk], [rk])
            self.stt(red[:, 0:TB], red[:, 0:TB], -TWO_PI, ang[:, 0:TB], ALU.mult, ALU.add, [rk, ak], [rk])
            self.ts(red[:, 0:TB], red[:, 0:TB], float(np.pi), float(-np.pi), ALU.min, ALU.max, [rk], [rk])
            self.act(dst, red[:, 0:TB], AF.Sin, [rk], [dk])

    def qk_norm_rope(self, ps, pk, gname, dst, dk):
        qsq, qk_ = self.rotbuf("tb", self.tb)
        self.act(qsq[:, 0:TB], ps, AF.Square, [pk], [qk_])
        pss, psk = self.bank()
        self.mm(pss, self.con("hones"), qsq[:, 0:TB], True, True, ["consb", qk_], [psk])
        sd, sk = self.rotbuf("tf", self.tf)
        self.act(sd[:, 0:TB], pss, AF.Ln, [psk, "misc"], [sk], bias=self.misc[:, 0:1], scale=1.0 / 64)
        self.act(sd[:, 0:TB], sd[:, 0:TB], AF.Exp, [sk], [sk], scale=-0.5)
        qn, qnk = self.rotbuf("tb", self.tb)
        self.stt(qn[:, 0:TB], ps, self.col(gname), sd[:, 0:TB], ALU.mult, ALU.mult, [pk, sk, "cols"], [qnk])
        psr, prk = self.bank()
        self.mm(psr, self.con("rt"), qn[:, 0:TB], True, True, ["consb", qnk], [prk])
        t1, t1k = self.rotbuf("tf", self.tf)
        t2, t2k = self.rotbuf("tf", self.tf)
        self.tt(t1[:, 0:TB], qn[:, 0:TB], self.cosb, ALU.mult, [qnk, "cos"], [t1k], eng="pool")
        self.tt(t2[:, 0:TB], psr, self.sinb, ALU.mult, [prk, "sin"], [t2k])
        if isinstance(dst, tuple):
            d0, d1 = dst
            self.tt(d0[0:64], t1[0:64, 0:TB], t2[0:64, 0:TB], ALU.add, [t1k, t2k], [dk], eng="pool")
            self.tt(d1[64:128], t1[64:128, 0:TB], t2[64:128, 0:TB], ALU.add, [t1k, t2k], [dk], eng="pool")
        else:
            self.tt(dst, t1[:, 0:TB], t2[:, 0:TB], ALU.add, [t1k, t2k], [dk], eng="pool")

    def mixer1(self, blk, xb, xk):
        self.rmsnorm(xb, xk, "nmix1")
        for g in range(2):
            wv, wk = self.getw("wq%d" % g)
            for m in range(4):
                c = g * 4 + m
                ps, pk = self.proj(wv, wk, m * 128, self.h, "h", 8)
                self.qk_norm_rope(ps, pk, "qn", self.qrope[:, c, :], ("qk8", c))
            self.donew()
        wv, wk = self.getw("wkd")
        for g in range(4):
            ps, pk = self.proj(wv, wk, g * 128, self.h, "h", 8)
            self.qk_norm_rope(ps, pk, "kn", (self.kbuf[:, 2 * g, 128:128 + TB], self.kbuf[:, 2 * g + 1, 128:128 + TB]), ("kbuf", g))
        self.donew()
        wv, wk = self.getw("wv")
        for tt_ in range(NT):
            ps, pk = self.bank()
            for kc in range(8):
                self.mm(ps[:, 0:256], self.h[:, kc, tt_ * 128:(tt_ + 1) * 128], wv[:, kc, :], kc == 0, kc == 7, ["h", wk], [pk])
            self.cp(self.vbuf[:, 1 + tt_, :, 0:64], ps[:, 0:256].rearrange("p (g e) -> p g e", g=4), [pk], ["vbuf"], eng="act")
        self.donew()
        so, _ = ROWS["snk"]
        for tt_ in range(NT):
            first = (blk == 0 and tt_ == 0)
            yt, ytk = self.rotbuf("ytok", self.ytok)
            for cp_ in range(4):
                g = cp_
                pes = []
                for j in range(2):
                    pss, psk = self.bank()
                    for ci in range(2):
                        c = 2 * cp_ + ci
                        q = self.qrope[:, c, tt_ * 128:(tt_ + 1) * 128]
                        for half in range(2):
                            o_ = pss[:, ci * 256 + half * 128:ci * 256 + half * 128 + 128]
                            kcol = half * 128 + tt_ * 128
                            self.mm(o_, self.kbuf[:, 2 * g + j, kcol:kcol + 128], q, True, False, [("kbuf", g), ("qk8", c)], [psk])
                            self.mm(o_, self.con("ident"), self.mask4[:, half * 128:(half + 1) * 128], False, True,
                                    ["consb", "mask4"], [psk])
                    pe_, pek = self.rotbuf("tb", self.tb)
                    self.act(pe_[:, 0:512], pss, AF.Exp, [psk], [pek], scale=0.125)
                    pes.append((pe_, pek))
                pso, pok = self.bank()
                for ci in range(2):
                    for j in range(2):
                        idx = ci * 2 + j
                        pe_, pek = pes[j]
                        o_ = pso[:, idx * 65:(idx + 1) * 65]
                        if not first:
                            self.mm(o_, pe_[:, ci * 256:ci * 256 + 128], self.vbuf[:, tt_, g, :], True, False, [pek, "vbuf"], [pok])
                        self.mm(o_, pe_[:, ci * 256 + 128:ci * 256 + 256], self.vbuf[:, tt_ + 1, g, :], first, True, [pek, "vbuf"], [pok])
                sm, smk = self.rotbuf("sm", self.sm)
                self.tt(sm[:, 0:4], pso[:, 0:260].rearrange("p (j e) -> p j e", j=4)[:, :, 64],
                        self.esink[:, 4 * cp_:4 * cp_ + 4], ALU.add, [pok, "esink"], [smk])
                self.rcp(sm[:, 4:8], sm[:, 0:4], [smk], [smk])
                for idx in range(4):
                    hq = 4 * cp_ + idx
                    self.ts(yt[:, hq * 64:(hq + 1) * 64], pso[:, idx * 65:idx * 65 + 64], sm[:, 4 + idx:5 + idx], None, ALU.mult, None,
                            [pok, smk], [ytk])
            psy, pyk = self.bank()
            psyb = psy.bitcast(BF16)
            for c in range(8):
                self.tr(psyb[:, c * 128:(c + 1) * 128], yt[:, c * 128:(c + 1) * 128], self.con("ident"), [ytk, "consb"], [pyk])
            self.cp(self.yfm[:, :, tt_ * 128:(tt_ + 1) * 128], psyb.rearrange("p (c t) -> p c t", c=8), [pyk], [("yfm", q_) for q_ in range(8)], eng="act")
        for g in range(4):
            self.cp(self.kbuf[:, 2 * g:2 * g + 2, 0:128], self.kbuf[:, 2 * g:2 * g + 2, TB:TB + 128], [("kbuf", g)], [("kbuf", g)])
        self.cp(self.vbuf[:, 0, :, :], self.vbuf[:, NT, :, :], ["vbuf"], ["vbuf"])
        for g in range(2):
            wv, wk = self.getw("atout%d" % g)
            for m in range(4):
                ps, pk = self.proj(wv, wk, m * 128, self.yfm, [("yfm", q_) for q_ in range(8)], 8)
                mm_ = g * 4 + m
                self.tt(xb[:, mm_, :], xb[:, mm_, :], ps, ALU.add, [(xk, mm_), pk], [(xk, mm_)])
            self.donew()

    def build(self):
        d = self.d
        self.plan_loads()
        self.setup()
        for _ in range(NSLOT):
            self.issue_load()
        xT = d["xT"].rearrange("(c p) t -> p c t", p=128)
        oT = d["outT"].rearrange("(c p) t -> p c t", p=128)
        ns = self.nstage
        outkeys = []
        def load_x(b_):
            self.dma("sp", self.x[b_ % 2], xT[:, :, b_ * TB:(b_ + 1) * TB], "xin%d" % (b_ % 2), (), [(("x", b_ % 2), m_) for m_ in range(8)])

        def load_p(b_):
            for l_ in range(2):
                if ns >= 3 + 3 * l_:
                    self.dma("pool", self.pblk[l_], d["pT"][l_].rearrange("(k p) t -> p k t", p=128)[:, :, b_ * TB:(b_ + 1) * TB],
                             "psem%d" % l_, (), [("pblk", l_)])

        load_x(0)
        for blk in range(self.nblk):
            par = blk % 2
            t0 = blk * TB
            xb, xk = self.x[par], ("x", par)
            if blk + 1 < self.nblk:
                load_x(blk + 1)
            load_p(blk)
            if ns >= 4 and "mix1" not in self.skip:
                self.rope_tables(blk)
            for l in range(2):
                if l == 0 and ns >= 1 and "mix0" not in self.skip:
                    self.mixer0(blk, xb, xk)
                if l == 1 and ns >= 4 and "mix1" not in self.skip:
                    self.mixer1(blk, xb, xk)
                if ns >= 2 + 3 * l and ("ffn%d" % l) not in self.skip:
                    self.ffn(l, xb, xk)
                if ns >= 3 + 3 * l and ("ple%d" % l) not in self.skip:
                    self.ple(l, blk, xb, xk)
            self.dma("sp", oT[:, :, t0:t0 + TB], xb, "xout%d" % par, [(xk, m_) for m_ in range(8)], [("out", blk)])
            outkeys.append(("out", blk))
        assert self.nload == len(self.loads), (self.nload, len(self.loads))
        self.s.final_wait("sp", outkeys + self.dbgkeys)
        nc = self.nc
        sch = self.s
        sch.finalize(reorder=self.reorder, keep=self.keep)
        with nc.Block() as block:
            @block.tensor
            def _(e):
                sch.replay("pe", e)

            @block.scalar
            def _(e):
                sch.replay("act", e)

            @block.vector
            def _(e):
                sch.replay("dve", e)

            @block.gpsimd
            def _(e):
                sch.replay("pool", e)

            @block.sync
            def _(e):
                sch.replay("sp", e)
        return nc


def kernel(**inputs):
    x = np.asarray(inputs["x"], np.float32)
    p = np.asarray(inputs["p"], np.float32)
    pos = np.asarray(inputs["positions"], np.int32)
    shared = host_tables(inputs)
    nb = x.shape[0]
    in_maps = []
    for b in range(nb):
        m = dict(shared)
        m["xT"] = np.ascontiguousarray(x[b].T)
        m["pT"] = np.ascontiguousarray(p[:, b].transpose(0, 2, 1))
        m["pos"] = np.ascontiguousarray(pos[b].reshape(1, S))
        in_maps.append(m)
    prog = Prog(nblk=8, nstage=6)
    nc = prog.build()
    res = run_bass_kernel_spmd(nc, in_maps, core_ids=list(range(nb)))
    out = np.stack([np.ascontiguousarray(r["outT"].T) for r in res.results], axis=0)
    return out.astype(np.float32)
```
